# Optimizing a Trainium2 kernel written in Bass

```python
import math
import jax, jax.numpy as jnp
from jax import lax
import numpy as np

D_MODEL = 1024
BATCH = 8
SEQ = 4096
DEPTH = 2

N_MIXERS = 2
EPS = 1e-6
MEM_LEN = 256

MIX_WIDTH = D_MODEL
MEM_HEADS = 4
MEM_HEAD_DIM = 64
MEM_WIDTH = MEM_HEADS * MEM_HEAD_DIM
SEQ_WIDTH = MIX_WIDTH - MEM_WIDTH

A_HEAD_DIM = 64
A_HEADS = SEQ_WIDTH // A_HEAD_DIM
A_KV_RANK = 256
IDX_HEADS = 8
IDX_DIM = 64
IDX_TOPK_MAX = 256
Q_BLOCK = 128

REL_BUCKETS = 32
REL_MAX_DIST = 128

SSM_HEAD_DIM = 64
SSM_HEADS = SEQ_WIDTH // SSM_HEAD_DIM
SSM_GROUPS = 2
HEADS_PER_GROUP = SSM_HEADS // SSM_GROUPS
SSM_STATE = 128
CONV_WIDTH = 4
SSD_CHUNK = 128
CONV_DIM = SEQ_WIDTH + 2 * SSM_GROUPS * SSM_STATE

PEER_HEADS = 8
PEER_KEYS = 128
PEER_EXPERTS = PEER_KEYS * PEER_KEYS
PEER_QDIM = 256
PEER_TOPK = 16
TOKEN_BLOCK = 128

N_A_LAYERS = (DEPTH + 1) // 2
N_B_LAYERS = DEPTH // 2


def _split_points(widths):
    pts, acc = [], 0
    for w in widths[:-1]:
        acc += w
        pts.append(acc)
    return pts


A_WIDTHS = [SEQ_WIDTH, A_KV_RANK, IDX_HEADS * IDX_DIM, IDX_DIM, IDX_HEADS, MEM_WIDTH]
B_WIDTHS = [SEQ_WIDTH, CONV_DIM, SSM_HEADS, MEM_WIDTH]
A_IN = sum(A_WIDTHS)
B_IN = sum(B_WIDTHS)
A_SPLITS = _split_points(A_WIDTHS)
B_SPLITS = _split_points(B_WIDTHS)

kernel_name = "hybrid_dsa_ssd_peer_trunk"


def rms_norm(x, g):
    xf = x.astype(jnp.float32)
    y = xf * lax.rsqrt(jnp.mean(xf * xf, axis=-1, keepdims=True) + EPS)
    return (y * g.astype(jnp.float32)).astype(x.dtype)


def t5_causal_bucket(dist):
    n = jnp.maximum(dist, 0)
    max_exact = REL_BUCKETS // 2
    nf = jnp.maximum(n, max_exact).astype(jnp.float32)
    large = max_exact + (jnp.log(nf / max_exact) / math.log(REL_MAX_DIST / max_exact)
                         * (REL_BUCKETS - max_exact)).astype(jnp.int32)
    large = jnp.minimum(large, REL_BUCKETS - 1)
    return jnp.where(n < max_exact, n, large)


def memory_attention(q, mem_n, w_kv):
    b, s, _ = q.shape
    k, v = jnp.split(mem_n @ w_kv, 2, axis=-1)
    q = q.reshape(b, s, MEM_HEADS, MEM_HEAD_DIM)
    k = k.reshape(b, -1, MEM_HEADS, MEM_HEAD_DIM)
    v = v.reshape(b, -1, MEM_HEADS, MEM_HEAD_DIM)
    logits = jnp.einsum('bqhd,bmhd->bhqm', q, k).astype(jnp.float32) * (MEM_HEAD_DIM ** -0.5)
    p = jax.nn.softmax(logits, axis=-1).astype(v.dtype)
    o = jnp.einsum('bhqm,bmhd->bqhd', p, v)
    return o.reshape(b, s, MEM_WIDTH)


def dsa_attention(q, c_kv, iq, ik, iw, w_uk, w_uv, rel_bias):
    b, s = c_kv.shape[:2]
    topk = min(IDX_TOPK_MAX, s // 4)
    n_blk = s // Q_BLOCK
    key_pos = jnp.arange(s, dtype=jnp.int32)

    def block(i):
        t0 = i * Q_BLOCK
        qb = lax.dynamic_slice_in_dim(q, t0, Q_BLOCK, axis=1)
        iqb = lax.dynamic_slice_in_dim(iq, t0, Q_BLOCK, axis=1)
        iwb = lax.dynamic_slice_in_dim(iw, t0, Q_BLOCK, axis=1)
        q_pos = t0 + jnp.arange(Q_BLOCK, dtype=jnp.int32)
        rel = jax.nn.relu(jnp.einsum('bqhd,bsd->bqhs', iqb, ik).astype(jnp.float32) * (IDX_DIM ** -0.5))
        score = jnp.einsum('bqhs,bqh->bqs', rel, iwb.astype(jnp.float32) * (IDX_HEADS ** -0.5))
        causal = key_pos[None, :] <= q_pos[:, None]
        score = jnp.where(causal[None], score, -jnp.inf)
        _, sel = lax.top_k(score, topk)
        c_sel = jax.vmap(lambda c, idx: c[idx])(c_kv, sel)
        q_lat = jnp.einsum('bqhd,hcd->bqhc', qb, w_uk)
        logits = jnp.einsum('bqhc,bqkc->bhqk', q_lat, c_sel).astype(jnp.float32) * (A_HEAD_DIM ** -0.5)
        dist = q_pos[None, :, None] - sel
        bias = rel_bias[t5_causal_bucket(dist)]
        logits = logits + jnp.moveaxis(bias, -1, 1).astype(jnp.float32)
        logits = jnp.where((dist >= 0)[:, None], logits, -jnp.inf)
        p = jax.nn.softmax(logits, axis=-1).astype(c_sel.dtype)
        o_lat = jnp.einsum('bhqk,bqkc->bqhc', p, c_sel)
        o = jnp.einsum('bqhc,hcd->bqhd', o_lat, w_uv)
        return o.reshape(b, Q_BLOCK, SEQ_WIDTH)

    out = lax.map(block, jnp.arange(n_blk))
    return jnp.moveaxis(out, 0, 1).reshape(b, s, SEQ_WIDTH)


def causal_depthwise_conv(x, w, bias):
    c = x.shape[-1]
    y = lax.conv_general_dilated(x, w[:, None, :].astype(x.dtype), window_strides=(1,),
                                 padding=[(CONV_WIDTH - 1, 0)],
                                 dimension_numbers=('NWC', 'WIO', 'NWC'),
                                 feature_group_count=c)
    return y + bias.astype(x.dtype)


def ssd_scan(xh, dt, a, bm, cm):
    b, s, g, r, p = xh.shape
    n = bm.shape[-1]
    l = SSD_CHUNK
    c = s // l
    f32 = jnp.float32
    x = (xh.astype(f32) * dt[..., None]).reshape(b, c, l, g, r, p)
    da = (dt * a).reshape(b, c, l, g, r)
    bc = bm.astype(f32).reshape(b, c, l, g, n)
    cc = cm.astype(f32).reshape(b, c, l, g, n)
    a_cs = jnp.cumsum(da, axis=2)
    seg = a_cs[:, :, :, None] - a_cs[:, :, None, :]
    tri = jnp.tril(jnp.ones((l, l), dtype=bool))[None, None, :, :, None, None]
    decay = jnp.exp(jnp.where(tri, seg, -jnp.inf))
    cb = jnp.einsum('bclgn,bcsgn->bclsg', cc, bc)
    y_diag = jnp.einsum('bclsg,bclsgr,bcsgrp->bclgrp', cb, decay, x)
    decay_to_end = jnp.exp(a_cs[:, :, -1:] - a_cs)
    states = jnp.einsum('bclgn,bclgr,bclgrp->bcgrpn', bc, decay_to_end, x)
    chunk_decay = jnp.exp(a_cs[:, :, -1])

    def step(h, inp):
        st, dec = inp
        return h * dec[..., None, None] + st, h

    h0 = jnp.zeros((b, g, r, p, n), f32)
    _, h_prev = lax.scan(step, h0, (jnp.moveaxis(states, 1, 0), jnp.moveaxis(chunk_decay, 1, 0)))
    h_prev = jnp.moveaxis(h_prev, 0, 1)
    y_off = jnp.einsum('bclgn,bcgrpn,bclgr->bclgrp', cc, h_prev, jnp.exp(a_cs))
    return (y_diag + y_off).reshape(b, s, g, r, p)


def mamba2_mixer(z, xbc, dt_raw, conv_w, conv_b, dt_bias, a_log, d_skip, out_norm):
    b, s, _ = z.shape
    xbc = jax.nn.silu(causal_depthwise_conv(xbc, conv_w, conv_b))
    xs, bm, cm = jnp.split(xbc, [SEQ_WIDTH, SEQ_WIDTH + SSM_GROUPS * SSM_STATE], axis=-1)
    xh = xs.reshape(b, s, SSM_GROUPS, HEADS_PER_GROUP, SSM_HEAD_DIM)
    bm = bm.reshape(b, s, SSM_GROUPS, SSM_STATE)
    cm = cm.reshape(b, s, SSM_GROUPS, SSM_STATE)
    dt = jax.nn.softplus(dt_raw.astype(jnp.float32) + dt_bias.astype(jnp.float32))
    dt = dt.reshape(b, s, SSM_GROUPS, HEADS_PER_GROUP)
    a = -jnp.exp(a_log.astype(jnp.float32)).reshape(SSM_GROUPS, HEADS_PER_GROUP)
    d = d_skip.astype(jnp.float32).reshape(SSM_GROUPS, HEADS_PER_GROUP)
    y = ssd_scan(xh, dt, a, bm, cm) + xh.astype(jnp.float32) * d[..., None]
    y = y.reshape(b, s, SEQ_WIDTH) * jax.nn.silu(z.astype(jnp.float32))
    return rms_norm(y, out_norm).astype(z.dtype)


def peer_ffn(h, w_q, sub_keys, u, v):
    b, s, d = h.shape
    q = (h @ w_q).reshape(b, s, PEER_HEADS, 2, PEER_QDIM // 2)
    scores = jnp.einsum('bshic,ikc->bshik', q, sub_keys).astype(jnp.float32)
    top_s, top_i = lax.top_k(scores, PEER_TOPK)
    cand_s = top_s[..., 0, :, None] + top_s[..., 1, None, :]
    cand_i = top_i[..., 0, :, None] * PEER_KEYS + top_i[..., 1, None, :]
    cand_s = cand_s.reshape(b, s, PEER_HEADS, PEER_TOPK * PEER_TOPK)
    cand_i = cand_i.reshape(b, s, PEER_HEADS, PEER_TOPK * PEER_TOPK)
    best_s, pos = lax.top_k(cand_s, PEER_TOPK)
    expert = jnp.take_along_axis(cand_i, pos, axis=-1)
    gate = jax.nn.softmax(best_s, axis=-1).astype(h.dtype)
    nb = (b * s) // TOKEN_BLOCK
    hb = h.reshape(nb, TOKEN_BLOCK, d)
    eb = expert.reshape(nb, TOKEN_BLOCK, PEER_HEADS, PEER_TOPK)
    gb = gate.reshape(nb, TOKEN_BLOCK, PEER_HEADS, PEER_TOPK)

    def block(args):
        xt, et, gt = args
        act = jax.nn.gelu(jnp.einsum('td,thkd->thk', xt, u[et]), approximate=False)
        return jnp.einsum('thk,thkd->td', act * gt, v[et])

    out = lax.map(block, (hb, eb, gb))
    return out.reshape(b, s, d)


def setup_inputs(seed: int = 0) -> dict:
    key = jax.random.key(seed)
    ks = jax.random.split(key, 26)
    f32 = jnp.float32
    nA, nB = N_A_LAYERS, N_B_LAYERS

    def nrm(k, shape, scale):
        return jax.random.normal(k, shape, f32) * scale

    def gain(k, shape):
        return 1.0 + 0.05 * jax.random.normal(k, shape, f32)

    dt0 = jnp.exp(jax.random.uniform(ks[14], (nB, SSM_HEADS), f32, math.log(1e-3), math.log(1e-1)))
    return {
        "x": jax.random.normal(ks[0], (BATCH, SEQ, D_MODEL), f32),
        "mem": jax.random.normal(ks[1], (BATCH, MEM_LEN, D_MODEL), f32),
        "mem_norm": gain(ks[2], (D_MODEL,)),
        "rel_bias": nrm(ks[3], (REL_BUCKETS, A_HEADS), 0.5),
        "mix_norm": gain(ks[4], (DEPTH, D_MODEL)),
        "ffn_norm": gain(ks[5], (DEPTH, D_MODEL)),
        "final_norm": gain(ks[6], (D_MODEL,)),
        "w_o": nrm(ks[7], (DEPTH, MIX_WIDTH, D_MODEL), MIX_WIDTH ** -0.5),
        "w_mem_kv": nrm(ks[8], (DEPTH, D_MODEL, 2 * MEM_WIDTH), D_MODEL ** -0.5),
        "a_w_in": nrm(ks[9], (nA, D_MODEL, A_IN), D_MODEL ** -0.5),
        "a_kv_norm": gain(ks[10], (nA, A_KV_RANK)),
        "a_w_uk": nrm(ks[11], (nA, A_HEADS, A_KV_RANK, A_HEAD_DIM), A_KV_RANK ** -0.5),
        "a_w_uv": nrm(ks[12], (nA, A_HEADS, A_KV_RANK, A_HEAD_DIM), A_KV_RANK ** -0.5),
        "b_w_in": nrm(ks[13], (nB, D_MODEL, B_IN), D_MODEL ** -0.5),
        "b_conv_w": nrm(ks[15], (nB, CONV_WIDTH, CONV_DIM), CONV_WIDTH ** -0.5),
        "b_conv_b": nrm(ks[16], (nB, CONV_DIM), 0.02),
        "b_dt_bias": dt0 + jnp.log(-jnp.expm1(-dt0)),
        "b_a_log": jnp.log(jax.random.uniform(ks[17], (nB, SSM_HEADS), f32, 1.0, 16.0)),
        "b_d_skip": 1.0 + 0.1 * jax.random.normal(ks[18], (nB, SSM_HEADS), f32),
        "b_out_norm": gain(ks[19], (nB, SEQ_WIDTH)),
        "peer_w_q": nrm(ks[20], (DEPTH, D_MODEL, PEER_HEADS * PEER_QDIM), D_MODEL ** -0.5),
        "peer_sub_keys": nrm(ks[21], (DEPTH, 2, PEER_KEYS, PEER_QDIM // 2), (PEER_QDIM // 2) ** -0.5),
        "peer_u": nrm(ks[22], (DEPTH, PEER_EXPERTS, D_MODEL), D_MODEL ** -0.5),
        "peer_v": nrm(ks[23], (DEPTH, PEER_EXPERTS, D_MODEL), D_MODEL ** -0.5),
    }


def reference(x, mem, mem_norm, rel_bias, mix_norm, ffn_norm, final_norm, w_o, w_mem_kv,
              a_w_in, a_kv_norm, a_w_uk, a_w_uv,
              b_w_in, b_conv_w, b_conv_b, b_dt_bias, b_a_log, b_d_skip, b_out_norm,
              peer_w_q, peer_sub_keys, peer_u, peer_v):
    b, s, _ = x.shape
    mem_n = rms_norm(mem, mem_norm)
    for i in range(DEPTH):
        h = rms_norm(x, mix_norm[i])
        j = i // N_MIXERS
        if i % N_MIXERS == 0:
            q, c_kv, iq, ik, iw, q_mem = jnp.split(h @ a_w_in[j], A_SPLITS, axis=-1)
            c_kv = rms_norm(c_kv, a_kv_norm[j])
            seq_out = dsa_attention(q.reshape(b, s, A_HEADS, A_HEAD_DIM), c_kv,
                                    iq.reshape(b, s, IDX_HEADS, IDX_DIM), ik, iw,
                                    a_w_uk[j], a_w_uv[j], rel_bias)
        else:
            z, xbc, dt_raw, q_mem = jnp.split(h @ b_w_in[j], B_SPLITS, axis=-1)
            seq_out = mamba2_mixer(z, xbc, dt_raw, b_conv_w[j], b_conv_b[j], b_dt_bias[j],
                                   b_a_log[j], b_d_skip[j], b_out_norm[j])
        mem_out = memory_attention(q_mem, mem_n, w_mem_kv[i])
        x = x + jnp.concatenate([seq_out, mem_out], axis=-1) @ w_o[i]
        x = x + peer_ffn(rms_norm(x, ffn_norm[i]), peer_w_q[i], peer_sub_keys[i], peer_u[i], peer_v[i])
    return rms_norm(x, final_norm)
```

```python
import math
import numpy as np
from contextlib import ExitStack
import ml_dtypes
import concourse.bass as bass
import concourse.mybir as mybir
from concourse.bass_utils import run_bass_kernel_spmd

F32 = mybir.dt.float32
BF16 = mybir.dt.bfloat16
U32 = mybir.dt.uint32
AF = mybir.ActivationFunctionType
ALU = mybir.AluOpType
AX = mybir.AxisListType

ENGS = ("pe", "act", "dve", "pool", "sp")
NEG = -1.0e30

S = 4096
D = 1024
NT = S // 128
A_IN = 1864
B_IN = 2316


class V:
    def __init__(self, bufs, ap):
        self.bufs = bufs if isinstance(bufs, list) else [bufs]
        self.ap = ap

    def __getitem__(self, idx):
        return V(self.bufs, self.ap[idx])

    def bc(self, shape):
        return V(self.bufs, self.ap.broadcast_to(list(shape)))

    def us(self, axis):
        return V(self.bufs, self.ap.unsqueeze(axis))

    def re(self, pat, **kw):
        return V(self.bufs, self.ap.rearrange(pat, **kw))

    def bitcast(self, dt):
        return V(self.bufs, self.ap.bitcast(dt))

    def pbc(self, n):
        return V(self.bufs, self.ap.partition_broadcast(n))


class Buf:
    def __init__(self, t, name, space, view=None):
        self.t = t
        self.name = name
        self.space = space
        self.view = view if view is not None else (t.ap() if space == "dr" else t[:])
        self.w = []
        self.r = []
        self.dsem = None
        self.dcnt = 0

    def __getitem__(self, idx):
        return V(self, self.view[idx])

    def v(self):
        return V(self, self.view)


class K:
    def __init__(self, nc, es):
        self.nc = nc
        self.es = es
        self.ops = {e: [] for e in ENGS}
        self.sem = {e: es.enter_context(nc.semaphore("s_" + e)) for e in ENGS}
        self.cnt = {e: 0 for e in ENGS}
        self.waited = {e: {} for e in ENGS}
        self.dsems = []
        self.nbuf = 0
        self.free_dsems = []
        self.q = 0
        self.allbufs = []
        self.log = []

    def sb(self, shape, dtype, es=None, name=None):
        self.nbuf += 1
        name = name or f"sb{self.nbuf}"
        t = (es or self.es).enter_context(self.nc.sbuf_tensor(name, list(shape), dtype))
        b = Buf(t, name, "sb")
        self.allbufs.append(b)
        self.log.append(b)
        return b

    def reg(self, b):
        self.allbufs.append(b)
        return b

    def mark(self):
        return len(self.log)

    def release_since(self, m):
        self.release(self.log[m:])
        del self.log[m:]

    def dram(self, name, shape, dtype, kind="Internal"):
        t = self.nc.dram_tensor(name, list(shape), dtype, kind=kind)
        return self.reg(Buf(t, name, "dr"))

    def _dsem(self, b):
        if b.dsem is None:
            if self.free_dsems:
                b.dsem, b.dbase = self.free_dsems.pop()
            else:
                b.dsem = self.es.enter_context(self.nc.semaphore(f"d{len(self.dsems)}"))
                b.dbase = 0
            self.dsems.append(b)
        return b.dsem

    def release(self, bufs):
        for b in bufs:
            if b.dsem is not None:
                self.free_dsems.append((b.dsem, b.dbase + 16 * b.dcnt))
                self.dsems.remove(b)
                b.dsem = None

    def _need(self, eng, ev, waits, same_ok):
        if ev[0] == "E":
            _, e2, tk = ev
            if e2 == eng and same_ok:
                return
            key = ("E", e2)
            val = tk
            sem = self.sem[e2]
        else:
            owner = ev[1]
            if owner.dsem is None:
                return
            key = ("D", id(owner.dsem))
            val = owner.dbase + 16 * owner.dcnt
            sem = owner.dsem
        if self.waited[eng].get(key, 0) >= val:
            return
        self.waited[eng][key] = val
        waits.append((sem, val))

    def _deps(self, eng, reads, writes):
        waits = []
        for b in reads:
            for ev in b.w:
                self._need(eng, ev, waits, same_ok=(eng == "pe"))
        for b in writes:
            for ev in b.w:
                self._need(eng, ev, waits, same_ok=True)
            for ev in b.r:
                self._need(eng, ev, waits, same_ok=True)
        return waits

    def op(self, eng, fn, reads=(), writes=()):
        reads = list(dict.fromkeys(reads))
        writes = list(dict.fromkeys(writes))
        waits = self._deps(eng, reads, writes)
        self.cnt[eng] += 1
        ev = ("E", eng, self.cnt[eng])
        self.ops[eng].append((waits, fn, self.sem[eng], 1))
        for b in reads:
            b.r = [x for x in b.r if not (x[0] == "E" and x[1] == eng)] + [ev]
        for b in writes:
            b.w = [ev]
            b.r = []

    def dma(self, dst, src, q=None, **kw):
        if q is None:
            q = ("sp", "pool")[self.q % 2] if False else "sp"
        db, sbf = dst.bufs[0], src.bufs[0]
        owner = db if db.space != "dr" else sbf
        sem = self._dsem(owner)
        waits = self._deps(q, [sbf], [db])
        owner.dcnt += 1
        ev = ("D", owner)
        dap, sap = dst.ap, src.ap
        self.ops[q].append((waits, lambda e: e.dma_start(out=dap, in_=sap, **kw), sem, 16))
        if not any(x[0] == "D" and x[1] is owner for x in sbf.r):
            sbf.r.append(ev)
        if db.space == "dr":
            if not any(x[0] == "D" and x[1] is owner for x in db.w):
                db.w.append(ev)
        else:
            db.w = [ev]
        db.r = []

    def barrier(self, reset=()):
        for e in ENGS:
            waits = []
            for e2 in ENGS:
                if e2 != e and self.cnt[e2] > 0:
                    self._need(e, ("E", e2, self.cnt[e2]), waits, same_ok=False)
            for b in self.dsems:
                if b.dcnt:
                    self._need(e, ("D", b), waits, same_ok=False)
            if waits:
                self.ops[e].append((waits, None, None, 0))
        for b in self.allbufs:
            b.w = []
            b.r = []
        for e in ENGS:
            if self.cnt[e] > 30000:
                self.sem[e] = self.es.enter_context(self.nc.semaphore(f"s_{e}_{len(self.ops[e])}"))
                self.cnt[e] = 0
                for e2 in ENGS:
                    self.waited[e2].pop(("E", e), None)

    def emit(self, block):
        ops = self.ops

        def run(e, lst, attach):
            for waits, fn, sem, inc in lst:
                if fn is None or not attach:
                    for s, v in waits:
                        e.wait_ge(s, v)
                    if fn is not None:
                        fn(e).then_inc(sem, inc)
                    continue
                for s, v in waits[1:]:
                    e.wait_ge(s, v)
                ins = fn(e)
                if waits:
                    ins._wait_ge(waits[0][0], waits[0][1])
                ins.then_inc(sem, inc)

        @block.tensor
        def _(e):
            run(e, ops["pe"], True)

        @block.scalar
        def _(e):
            run(e, ops["act"], True)

        @block.vector
        def _(e):
            run(e, ops["dve"], True)

        @block.gpsimd
        def _(e):
            run(e, ops["pool"], True)

        @block.sync
        def _(e):
            run(e, ops["sp"], True)

    @staticmethod
    def _rw(outs, ins):
        r, w = [], []
        for x in ins:
            if isinstance(x, V):
                r += x.bufs
        for x in outs:
            if isinstance(x, V):
                w += x.bufs
        return r, w

    @staticmethod
    def _a(x):
        return x.ap if isinstance(x, V) else x

    def act(self, out, in_, func, bias=None, scale=1.0, accum=None):
        r, w = self._rw([out, accum], [in_, bias, scale])
        kw = {}
        if bias is not None:
            kw["bias"] = self._a(bias)
        if accum is not None:
            kw["accum_out"] = accum.ap
        sc = self._a(scale)
        self.op("act", lambda e: e.activation(out=out.ap, in_=in_.ap, func=func, scale=sc, **kw), r, w)

    def tt(self, eng, out, in0, in1, op):
        r, w = self._rw([out], [in0, in1])
        self.op(eng, lambda e: e.tensor_tensor(out=out.ap, in0=in0.ap, in1=in1.ap, op=op), r, w)

    def ts(self, eng, out, in0, s1, s2=None, op0=ALU.mult, op1=None, accum=None):
        r, w = self._rw([out, accum], [in0, s1, s2])
        kw = {}
        if op1 is not None:
            kw["op1"] = op1
        if accum is not None:
            kw["accum_out"] = accum.ap
        a1, a2 = self._a(s1), self._a(s2)
        self.op(eng, lambda e: e.tensor_scalar(out=out.ap, in0=in0.ap, scalar1=a1, scalar2=a2, op0=op0, **kw), r, w)

    def stt(self, eng, out, in0, scalar, in1, op0, op1):
        r, w = self._rw([out], [in0, scalar, in1])
        sc = self._a(scalar)
        self.op(eng, lambda e: e.scalar_tensor_tensor(out=out.ap, in0=in0.ap, scalar=sc, in1=in1.ap, op0=op0, op1=op1), r, w)

    def copy(self, eng, out, in_):
        r, w = self._rw([out], [in_])
        if eng == "act":
            self.op(eng, lambda e: e.activation(out=out.ap, in_=in_.ap, func=AF.Copy), r, w)
        else:
            self.op(eng, lambda e: e.tensor_copy(out=out.ap, in_=in_.ap), r, w)

    def memset(self, eng, out, val):
        self.op(eng, lambda e: e.memset(out.ap, val), [], out.bufs)

    def mm(self, out, lhsT, rhs, start=True, stop=True):
        r, w = self._rw([out], [lhsT, rhs])
        self.op("pe", lambda e: e.matmul(out.ap, lhsT=lhsT.ap, rhs=rhs.ap, start=start, stop=stop, skip_group_check=True), r, w)

    def tr(self, out, in_, ident):
        r, w = self._rw([out], [in_, ident])
        self.op("pe", lambda e: e.transpose(out=out.ap, in_=in_.ap, identity=ident.ap), r, w)

    def recip(self, out, in_):
        r, w = self._rw([out], [in_])
        self.op("dve", lambda e: e.reciprocal(out=out.ap, in_=in_.ap), r, w)


class Ctx:
    pass


def setup_common(k, c):
    nc = k.nc
    pst = k.es.enter_context(nc.psum_tensor("psall", [128, 4096], F32))
    c.psall = pst
    c.banks = [k.reg(Buf(pst, f"bank{b}", "ps", view=pst[:, b * 512:(b + 1) * 512])) for b in range(8)]
    c.nb = 0
    c.identf = k.sb([128, 128], F32)
    c.identb = k.sb([128, 128], BF16)
    k.dma(c.identf.v(), c.d["c_ident"].v())
    k.copy("dve", c.identb.v(), c.identf.v())
    c.eps = k.sb([128, 1], F32)
    k.memset("pool", c.eps.v(), 1e-6)
    c.one = k.sb([128, 1], F32)
    k.memset("pool", c.one.v(), 1.0)


def bank(c):
    b = c.banks[c.nb % 8]
    c.nb += 1
    return b


def psv(c, b0, n):
    return V([c.banks[b0 + i] for i in range(n)], c.psall[:, b0 * 512:(b0 + n) * 512])


def rmsnorm_tile(k, c, xin, gfull, out_bf, width, scr, ss, rs):
    k.act(scr, xin, AF.Square, accum=ss)
    k.act(rs, ss, AF.Sqrt, bias=c.eps.v(), scale=1.0 / width)
    k.recip(rs, rs)
    k.stt("dve", out_bf, xin, rs, gfull, ALU.mult, ALU.mult)


def load_w_bf16(k, c, es, dsrc, rows, cols, name, col0=0, ncols=None, stage=None):
    ncols = ncols or cols
    kc = rows // 128
    wt = k.sb([128, kc, ncols], BF16, es=es, name=name)
    with ExitStack() as es2:
        st = [k.sb([128, min(ncols, 2048)], F32, es=es2) for _ in range(2)]
        i = 0
        for kk in range(kc):
            for c0 in range(0, ncols, 2048):
                w_ = min(2048, ncols - c0)
                s = st[i % 2]
                i += 1
                k.dma(s[:, 0:w_], dsrc[kk * 128:(kk + 1) * 128, col0 + c0:col0 + c0 + w_])
                k.copy("pool" if i % 2 else "dve", wt[:, kk, c0:c0 + w_], s[:, 0:w_])
        k.barrier()
        k.release(st)
    return wt


def phase_mem(k, c):
    d = c.d
    c.kmT = [k.sb([64, 4, 256], BF16) for _ in range(2)]
    c.vm = [k.sb([128, 2, 4, 65], BF16) for _ in range(2)]
    with ExitStack() as es:
        gfull = k.sb([128, D], F32, es=es)
        k.dma(gfull.v(), d["mem_norm"].v().pbc(128))
        mnT = k.sb([128, 8, 256], BF16, es=es)
        xt = k.sb([128, D], F32, es=es)
        scr = k.sb([128, D], F32, es=es)
        hb = k.sb([128, D], BF16, es=es)
        ss = k.sb([128, 1], F32, es=es)
        rs = k.sb([128, 1], F32, es=es)
        for mt in range(2):
            k.dma(xt.v(), d["mem"][mt * 128:(mt + 1) * 128, :])
            rmsnorm_tile(k, c, xt.v(), gfull.v(), hb.v(), D, scr.v(), ss.v(), rs.v())
            b = bank(c)
            pb = b.v().bitcast(BF16)
            for kk in range(8):
                k.tr(pb[:, kk * 128:(kk + 1) * 128], hb[:, kk * 128:(kk + 1) * 128], c.identb.v())
            k.copy("dve", mnT[:, :, mt * 128:(mt + 1) * 128], pb.re("p (k t) -> p k t", k=8))
        for i in range(2):
            wkv = load_w_bf16(k, c, es, d["w_mem_kv"][i], D, 512, f"wkv{i}")
            for h in range(4):
                b = bank(c)
                for kk in range(8):
                    k.mm(b[0:64, 0:256], wkv[:, kk, h * 64:(h + 1) * 64], mnT[:, kk, :], start=(kk == 0), stop=(kk == 7))
                k.copy("act", c.kmT[i][:, h, :], b[0:64, 0:256])
            k.memset("pool", c.vm[i].v(), 1.0)
            for mt in range(2):
                b = bank(c)
                for kk in range(8):
                    k.mm(b[:, 0:256], mnT[:, kk, mt * 128:(mt + 1) * 128], wkv[:, kk, 256:512], start=(kk == 0), stop=(kk == 7))
                k.copy("act", c.vm[i][:, mt, :, 0:64], b[:, 0:256].re("p (h e) -> p h e", h=4))
        k.barrier()


def mem_attention(k, c, li, qm, nq, cat, wk):
    pt = wk["pt"]
    for h in range(4):
        ob = bank(c)
        for mt in range(2):
            sb_ = bank(c)
            k.mm(sb_[:, 0:nq * 128], c.kmT[li][:, h, mt * 128:(mt + 1) * 128], qm[:, h, :])
            p = pt[mt % 2]
            k.act(p[:, 0:nq * 128], sb_[:, 0:nq * 128], AF.Exp, scale=0.125)
            for jl in range(nq):
                k.mm(ob[:, jl * 65:(jl + 1) * 65], p[:, jl * 128:(jl + 1) * 128], c.vm[li][:, mt, h, :], start=(mt == 0 and jl == 0), stop=(mt == 1))
        rd = wk["rden"]
        ov = ob[:, 0:nq * 65].re("p (j e) -> p j e", e=65)
        k.recip(rd[:, 0:nq], ov[:, :, 64])
        k.tt("dve", cat[:, 0:nq, 768 + h * 64:768 + (h + 1) * 64], ov[:, :, 0:64], rd[:, 0:nq].us(2).bc([128, nq, 64]), ALU.mult)


def out_proj_residual(k, c, cat_t, wo, xin_dram, xout_dram, t0, wk):
    b = bank(c)
    pb = b.v().bitcast(BF16)
    for kk in range(8):
        k.tr(pb[:, kk * 128:(kk + 1) * 128], cat_t[:, kk * 128:(kk + 1) * 128], c.identb.v())
    catT = wk["catT"]
    k.copy("act", catT.v(), pb)
    xt = wk["xres"][wk["xi"] % 2]
    wk["xi"] += 1
    k.dma(xt.v(), xin_dram[t0:t0 + 128, :])
    for half in range(2):
        ob = bank(c)
        for kk in range(8):
            k.mm(ob.v(), catT[:, kk * 128:(kk + 1) * 128], wo[:, kk, half * 512:(half + 1) * 512], start=(kk == 0), stop=(kk == 7))
        k.tt("dve", xt[:, half * 512:(half + 1) * 512], xt[:, half * 512:(half + 1) * 512], ob.v(), ALU.add)
    k.dma(xout_dram[t0:t0 + 128, :], xt.v())


def layer0_mixer(k, c, xin, xout):
    d = c.d
    nc = k.nc
    S_ = c.S
    NT_ = S_ // 128
    NB = S_ // 512
    qT = k.dram("qT", [12, 64, S_], BF16)
    kT = k.dram("kT", [12, 64, S_], BF16)
    iqT = k.dram("iqT", [8, 64, S_], BF16)
    qmT = k.dram("qmT", [4, 64, S_], BF16)
    MT = k.dram("MT", [NT_, 128, NT_ * 128], BF16)
    with ExitStack() as esL:
        Vres = k.sb([128, NT_, 12, 65], BF16, es=esL)
        ikT = k.sb([64, S_], BF16, es=esL)
        absw = k.sb([128, NT_, 8], F32, es=esL)
        sgnw = k.sb([128, NT_, 8], F32, es=esL)
        k.memset("pool", Vres.v(), 1.0)
        with ExitStack() as es:
            win = load_w_bf16(k, c, es, d["a_w_in"], D, A_IN, "a_win")
            wuk = k.sb([128, 2, 12, 64], BF16, es=es)
            wuv = k.sb([128, 2, 12, 64], BF16, es=es)
            with ExitStack() as es2:
                st = k.sb([128, 2, 12, 64], F32, es=es2)
                for (dst, nm) in ((wuk, "a_w_uk"), (wuv, "a_w_uv")):
                    for cc in range(2):
                        k.dma(st[:, cc, :, :], d[nm][:, cc * 128:(cc + 1) * 128, :].re("h c e -> c h e"))
                    k.copy("dve", dst.v(), st.v())
                k.barrier()
                k.release([st])
            gfull = k.sb([128, D], F32, es=es)
            k.dma(gfull.v(), d["mix_norm"][0, :].pbc(128))
            gkv = k.sb([128, 256], F32, es=es)
            k.dma(gkv.v(), d["a_kv_norm"][0, :].pbc(128))
            xt = [k.sb([128, D], F32, es=es) for _ in range(2)]
            scr = k.sb([128, D], F32, es=es)
            hb = k.sb([128, D], BF16, es=es)
            ss = k.sb([128, 1], F32, es=es)
            rs = k.sb([128, 1], F32, es=es)
            hT = k.sb([128, 8, 512], BF16, es=es)
            cT = k.sb([128, 2, 512], BF16, es=es)
            ckv = k.sb([128, 256], F32, es=es)
            cn = k.sb([128, 256], BF16, es=es)
            iwt = k.sb([128, 8], F32, es=es)
            stg = [k.sb([64, 512], BF16, es=es) for _ in range(4)]
            si = 0
            for blk in range(NB):
                for tl in range(4):
                    j = blk * 4 + tl
                    x_ = xt[j % 2]
                    k.dma(x_.v(), xin[j * 128:(j + 1) * 128, :])
                    rmsnorm_tile(k, c, x_.v(), gfull.v(), hb.v(), D, scr.v(), ss.v(), rs.v())
                    b = bank(c)
                    pb = b.v().bitcast(BF16)
                    for kk in range(8):
                        k.tr(pb[:, kk * 128:(kk + 1) * 128], hb[:, kk * 128:(kk + 1) * 128], c.identb.v())
                    k.copy("act", hT[:, :, tl * 128:(tl + 1) * 128], pb.re("p (k t) -> p k t", k=8))
                    b = bank(c)
                    for kk in range(8):
                        k.mm(b[:, 0:256], hT[:, kk, tl * 128:(tl + 1) * 128], win[:, kk, 768:1024], start=(kk == 0), stop=(kk == 7))
                    for kk in range(8):
                        k.mm(b[:, 256:264], hT[:, kk, tl * 128:(tl + 1) * 128], win[:, kk, 1600:1608], start=(kk == 0), stop=(kk == 7))
                    k.copy("act", ckv.v(), b[:, 0:256])
                    k.copy("dve", iwt.v(), b[:, 256:264])
                    k.act(sgnw[:, j, :], iwt.v(), AF.Sign)
                    k.act(absw[:, j, :], iwt.v(), AF.Abs, scale=(8 ** -0.5) / 8.0)
                    k.act(scr[:, 0:256], ckv.v(), AF.Square, accum=ss.v())
                    k.act(rs.v(), ss.v(), AF.Sqrt, bias=c.eps.v(), scale=1.0 / 256)
                    k.recip(rs.v(), rs.v())
                    k.stt("dve", cn.v(), ckv.v(), rs.v(), gkv.v(), ALU.mult, ALU.mult)
                    b = bank(c)
                    pb = b.v().bitcast(BF16)
                    for cc in range(2):
                        k.tr(pb[:, cc * 128:(cc + 1) * 128], cn[:, cc * 128:(cc + 1) * 128], c.identb.v())
                    k.copy("act", cT[:, :, tl * 128:(tl + 1) * 128], pb[:, 0:256].re("p (k t) -> p k t", k=2))
                    b0 = bank(c)
                    b1 = bank(c)
                    for cc in range(2):
                        k.mm(b0.v(), cT[:, cc, tl * 128:(tl + 1) * 128], wuv[:, cc, 0:8, :].re("p h e -> p (h e)"), start=(cc == 0), stop=(cc == 1))
                    for cc in range(2):
                        k.mm(b1[:, 0:256], cT[:, cc, tl * 128:(tl + 1) * 128], wuv[:, cc, 8:12, :].re("p h e -> p (h e)"), start=(cc == 0), stop=(cc == 1))
                    k.copy("act", Vres[:, j, 0:8, 0:64], b0.v().re("p (h e) -> p h e", h=8))
                    k.copy("dve", Vres[:, j, 8:12, 0:64], b1[:, 0:256].re("p (h e) -> p h e", h=4))
                outs = []
                for h in range(12):
                    outs.append((h * 64, qT[h]))
                for h in range(8):
                    outs.append((1024 + h * 64, iqT[h]))
                outs.append((1536, None))
                for h in range(4):
                    outs.append((1608 + h * 64, qmT[h]))
                for (c0, dst) in outs:
                    b = bank(c)
                    for kk in range(8):
                        k.mm(b[0:64, :], win[:, kk, c0:c0 + 64], hT[:, kk, :], start=(kk == 0), stop=(kk == 7))
                    if dst is None:
                        k.copy("act", ikT[:, blk * 512:(blk + 1) * 512], b[0:64, :])
                    else:
                        s_ = stg[si % 4]
                        si += 1
                        k.copy("act" if si % 2 else "dve", s_.v(), b[0:64, :])
                        k.dma(dst[:, blk * 512:(blk + 1) * 512], s_.v())
                for h in range(12):
                    b = bank(c)
                    for cc in range(2):
                        k.mm(b[0:64, :], wuk[:, cc, h, :], cT[:, cc, :], start=(cc == 0), stop=(cc == 1))
                    s_ = stg[si % 4]
                    si += 1
                    k.copy("act" if si % 2 else "dve", s_.v(), b[0:64, :])
                    k.dma(kT[h][:, blk * 512:(blk + 1) * 512], s_.v())
            k.barrier(reset=[qT, kT, iqT, qmT])
            k.release(xt + stg)
        with ExitStack() as es:
            cb = k.sb([128, 128], F32, es=es)
            k.dma(cb.v(), d["c_cbias"].v())
            GRP = 4
            acc = [k.sb([128, S_], F32, es=es) for _ in range(GRP)]
            iq = [k.sb([64, 8, 128], BF16, es=es) for _ in range(2)]
            rl = [k.sb([128, 512], F32, es=es) for _ in range(3)]
            junk = k.sb([128, S_], BF16, es=es)
            mk = [k.sb([128, S_], BF16, es=es) for _ in range(2)]
            mst = [k.sb([128, S_], BF16, es=es) for _ in range(2)]
            smt = [k.sb([128, 8], F32, es=es) for _ in range(GRP)]
            ri = 0
            for g0 in range(0, NT_, GRP):
                tiles = list(range(g0, min(g0 + GRP, NT_)))
                for j in tiles:
                    a = acc[j % GRP]
                    q_ = iq[j % 2]
                    k.dma(q_.v(), iqT[:, :, j * 128:(j + 1) * 128].re("h e t -> e h t"))
                    W = (j + 1) * 128
                    for k0 in range(0, W, 512):
                        w_ = min(512, W - k0)
                        for h in range(8):
                            b = bank(c)
                            k.mm(b[:, 0:w_], q_[:, h, :], ikT[:, k0:k0 + w_])
                            r_ = rl[ri % 3]
                            ri += 1
                            k.act(r_[:, 0:w_], b[:, 0:w_], AF.Relu, scale=absw[:, j, h:h + 1])
                            if h == 0:
                                k.ts("dve", a[:, k0:k0 + w_], r_[:, 0:w_], sgnw[:, j, 0:1], None, op0=ALU.mult)
                            else:
                                k.stt("dve", a[:, k0:k0 + w_], r_[:, 0:w_], sgnw[:, j, h:h + 1], a[:, k0:k0 + w_], ALU.mult, ALU.add)
                bis = [j for j in tiles if (j + 1) * 128 > c.topk]
                for j in tiles:
                    a = acc[j % GRP]
                    W = (j + 1) * 128
                    jl = j % GRP
                    if (j + 1) * 128 > c.topk:
                        k.op("dve", (lambda a=a, W=W, jl=jl: lambda e: e.tensor_reduce(out=smt[jl][:, 5:6].ap, in_=a[:, 0:W].ap, axis=AX.X, op=ALU.max))(), [a], [smt[jl]])
                        k.op("dve", (lambda a=a, W=W, jl=jl: lambda e: e.tensor_reduce(out=smt[jl][:, 6:7].ap, in_=a[:, 0:W].ap, axis=AX.X, op=ALU.min))(), [a], [smt[jl]])
                        k.ts("dve", smt[jl][:, 0:1], smt[jl][:, 6:7], -1e-3, None, op0=ALU.add)
                        k.stt("dve", smt[jl][:, 1:2], smt[jl][:, 5:6], 2e-3, smt[jl][:, 6:7], ALU.add, ALU.subtract)
                    else:
                        k.memset("dve", smt[jl][:, 0:1], -1.0e29)
                    k.tt("pool", a[:, j * 128:W], a[:, j * 128:W], cb.v(), ALU.add)
                NIT = 15
                for it in range(NIT):
                    cf = 2.0 ** -(it + 1)
                    for j in bis:
                        jl = j % GRP
                        k.stt("dve", smt[jl][:, 2:3], smt[jl][:, 1:2], cf, smt[jl][:, 0:1], ALU.mult, ALU.add)
                    for j in bis:
                        jl = j % GRP
                        W = (j + 1) * 128
                        k.ts("dve", junk[:, 0:W], acc[jl][:, 0:W], smt[jl][:, 2:3], None, op0=ALU.is_ge, op1=ALU.add, accum=smt[jl][:, 3:4])
                    for j in bis:
                        jl = j % GRP
                        k.ts("dve", smt[jl][:, 4:5], smt[jl][:, 3:4], c.topk - 0.5, cf, op0=ALU.is_ge, op1=ALU.mult)
                    for j in bis:
                        jl = j % GRP
                        k.stt("dve", smt[jl][:, 0:1], smt[jl][:, 4:5], smt[jl][:, 1:2], smt[jl][:, 0:1], ALU.mult, ALU.add)
                for j in tiles:
                    jl = j % GRP
                    W = (j + 1) * 128
                    m_ = mk[j % 2]
                    ms = mst[j % 2]
                    k.ts("dve", m_[:, 0:W], acc[jl][:, 0:W], smt[jl][:, 0:1], None, op0=ALU.is_ge)
                    if c.dbg == "idx":
                        dbt = k.sb([128, 16], F32, es=es, name=f"dbt{j}")
                        k.copy("dve", dbt[:, 0:8], smt[jl].v())
                        k.op("dve", (lambda o=dbt[:, 8:9], x=m_[:, 0:W]: lambda e: e.tensor_reduce(out=o.ap, in_=x.ap, axis=AX.X, op=ALU.add))(), [m_], [dbt])
                        k.ts("dve", junk[:, 0:W], acc[jl][:, 0:W], smt[jl][:, 0:1], None, op0=ALU.is_ge, op1=ALU.add, accum=dbt[:, 9:10])
                        k.dma(xout[j * 128:(j + 1) * 128, 0:16], dbt.v())
                        k.dma(xout[j * 128:(j + 1) * 128, 16:16 + min(W, 1008)], acc[jl][:, 0:min(W, 1008)])
                    for i0 in range(0, j + 1, 8):
                        n_ = min(8, j + 1 - i0)
                        b = bank(c)
                        pb = b.v().bitcast(BF16)
                        for ii in range(n_):
                            k.tr(pb[:, ii * 128:(ii + 1) * 128], m_[:, (i0 + ii) * 128:(i0 + ii + 1) * 128], c.identb.v())
                        k.copy("act", ms[:, i0 * 128:(i0 + n_) * 128], pb[:, 0:n_ * 128])
                    k.dma(MT[j][:, 0:W], ms[:, 0:W])
            k.barrier(reset=[MT])
            k.release(iq + mst)
        if c.dbg == "idx":
            with ExitStack() as es:
                mb_ = k.sb([128, 1024], BF16, es=es)
                mf_ = k.sb([128, 1024], F32, es=es)
                k.dma(mb_.v(), MT[7][:, 0:1024])
                k.copy("dve", mf_.v(), mb_.v())
                k.dma(xout[0:128, :], mf_.v())
                k.barrier()
            return
        with ExitStack() as es:
            wo = load_w_bf16(k, c, es, d["w_o"][0], D, D, "wo0")
            rb = k.sb([32, 12], F32, es=es)
            k.dma(rb.v(), d["rel_bias"].v())
            ohr = k.sb([32, 512], F32, es=es)
            k.dma(ohr.v(), d["c_ohr"].v())
            oh31 = k.sb([32, 128], F32, es=es)
            k.dma(oh31.v(), d["c_oh31"].v())
            b31 = k.sb([128, 12], F32, es=es)
            b = bank(c)
            k.mm(b[:, 0:12], oh31.v(), rb.v())
            k.copy("dve", b31.v(), b[:, 0:12])
            Tn = [k.sb([128, 12, 128], BF16, es=es) for _ in range(2)]
            for kk in range(2):
                base = 0 if kk == 0 else 4
                pv = psv(c, base, 4)
                for t in range(128):
                    o0 = 255 - 128 * kk - t
                    k.mm(V([c.banks[base + t // 32]], c.psall[:, base * 512 + t * 16: base * 512 + t * 16 + 12]), ohr[:, o0:o0 + 128], rb.v())
                k.act(Tn[kk].v().re("p h t -> p t h"), pv.re("p (t e) -> p t e", e=16)[:, :, 0:12], AF.Exp)
            qh = [k.sb([64, 512], BF16, es=es) for _ in range(2)]
            kh = [k.sb([64, S_], BF16, es=es) for _ in range(2)]
            qm = k.sb([64, 4, 512], BF16, es=es)
            mkJ = [k.sb([128, NT_, 512], BF16, es=es) for _ in range(1)]
            pt = [k.sb([128, 512], BF16, es=es) for _ in range(3)]
            cat = k.sb([128, 4, D], BF16, es=es)
            rden = k.sb([128, 4], F32, es=es)
            wk = {"pt": pt, "rden": rden, "catT": k.sb([128, D], BF16, es=es),
                  "xres": [k.sb([128, D], F32, es=es) for _ in range(2)], "xi": 0}
            pi = 0
            hi = 0
            sbi = 0
            for J in range(NB):
                nst = 4 * J + 4
                mJ = mkJ[0]
                for jl in range(4):
                    jj = 4 * J + jl
                    k.dma(mJ[:, 0:jj + 1, jl * 128:(jl + 1) * 128], MT[jj][:, 0:(jj + 1) * 128].re("p (i t) -> p i t", t=128))
                for h in range(12):
                    q_ = qh[hi % 2]
                    k_ = kh[hi % 2]
                    hi += 1
                    k.dma(q_.v(), qT[h][:, J * 512:(J + 1) * 512])
                    k.dma(k_[:, 0:nst * 128], kT[h][:, 0:nst * 128])
                    ob = c.banks[hi % 2]
                    for i in range(nst):
                        dl = max(0, i - 4 * J)
                        c0 = dl * 128
                        sb_ = c.banks[2 + sbi % 6]
                        sbi += 1
                        k.mm(sb_[:, c0:512], k_[:, i * 128:(i + 1) * 128], q_[:, c0:512])
                        p = pt[pi % 3]
                        pi += 1
                        k.act(p[:, c0:512], sb_[:, c0:512], AF.Exp, bias=b31[:, h:h + 1], scale=0.125)
                        k.tt("dve", p[:, c0:512], p[:, c0:512], mJ[:, i, c0:512], ALU.mult)
                        for jl in range(dl, 4):
                            jj = 4 * J + jl
                            if jj - i <= 1:
                                k.tt("pool", p[:, jl * 128:(jl + 1) * 128], p[:, jl * 128:(jl + 1) * 128], Tn[jj - i][:, h, :], ALU.mult)
                        for jl in range(dl, 4):
                            jj = 4 * J + jl
                            k.mm(ob[:, jl * 65:(jl + 1) * 65], p[:, jl * 128:(jl + 1) * 128], Vres[:, i, h, :], start=(i == 0 and jl == 0), stop=(i == jj))
                    ov = ob[:, 0:260].re("p (j e) -> p j e", e=65)
                    if c.dbg == "cat" and J == 1 and h == 0:
                        dbo = k.sb([128, 1024], F32, es=es, name="dbo")
                        k.memset("pool", dbo.v(), 0.0)
                        k.copy("dve", dbo[:, 0:260], ob[:, 0:260])
                        k.copy("dve", dbo[:, 512:1024], p.v())
                        k.dma(xout[0:128, :], dbo.v())
                        dbo2 = k.sb([128, 1024], F32, es=es, name="dbo2")
                        k.copy("dve", dbo2[:, 0:512], mJ[:, 7, :])
                        k.copy("dve", dbo2[:, 512:1024], mJ[:, 6, :])
                        k.dma(xout[128:256, :], dbo2.v())
                    k.recip(rden.v(), ov[:, :, 64])
                    k.tt("dve", cat[:, :, h * 64:(h + 1) * 64], ov[:, :, 0:64], rden.v().us(2).bc([128, 4, 64]), ALU.mult)
                k.dma(qm.v(), qmT[:, :, J * 512:(J + 1) * 512].re("h e t -> e h t"))
                mem_attention(k, c, 0, qm.v(), 4, cat.v(), wk)
                for jl in range(4):
                    if getattr(c, "dbg", None) == "cat":
                        xt_ = wk["xres"][jl % 2]
                        k.copy("dve", xt_.v(), cat[:, jl, :])
                        k.dma(xout[(4 * J + jl) * 128:(4 * J + jl + 1) * 128, :], xt_.v())
                        continue
                    out_proj_residual(k, c, cat[:, jl, :], wo, xin, xout, (4 * J + jl) * 128, wk)
            k.barrier(reset=[xout])
            k.release(qh + kh + [qm] + mkJ + wk["xres"])
    k.barrier()


def peer_layer(k, c, li, xin, xout, final):
    d = c.d
    S_ = c.S
    TB = 256
    NBK = S_ // TB
    NCH = 128
    uT = k.dram(f"uT{li}", [NCH, 128, 8, 128], BF16)
    vb = k.dram(f"vb{li}", [16384, D], BF16)
    lb = [4]

    def lbank():
        b = c.banks[4 + lb[0] % 4]
        lb[0] += 1
        return b

    with ExitStack() as es:
        m0 = k.mark()
        uf = [k.sb([128, 2, D], F32, es=es) for _ in range(2)]
        vf = [k.sb([128, 2, D], F32, es=es) for _ in range(2)]
        ubf = [k.sb([128, 2, D], BF16, es=es) for _ in range(2)]
        vbf = [k.sb([128, 2, D], BF16, es=es) for _ in range(2)]
        uo = [k.sb([128, 2, 8, 128], BF16, es=es) for _ in range(2)]
        for a2 in range(NCH // 2):
            u_, v_, ub_, vb_, uo_ = uf[a2 % 2], vf[a2 % 2], ubf[a2 % 2], vbf[a2 % 2], uo[a2 % 2]
            k.dma(u_.v(), d["peer_u"][li, a2 * 256:(a2 + 1) * 256, :].re("(a p) f -> p a f", a=2))
            k.dma(v_.v(), d["peer_v"][li, a2 * 256:(a2 + 1) * 256, :].re("(a p) f -> p a f", a=2))
            k.copy("dve", ub_.v(), u_.v())
            k.copy("act", vb_.v(), v_.v())
            for ai in range(2):
                b = bank(c)
                pb = b.v().bitcast(BF16)
                for kk in range(8):
                    k.tr(pb[:, kk * 128:(kk + 1) * 128], ub_[:, ai, kk * 128:(kk + 1) * 128], c.identb.v())
                k.copy("act" if ai else "dve", uo_[:, ai, :, :], pb.re("p (k e) -> p k e", k=8))
            k.dma(uT[a2 * 2:a2 * 2 + 2].re("a p k e -> p a k e"), uo_.v())
            k.dma(vb[a2 * 256:(a2 + 1) * 256, :].re("(a p) f -> p a f", a=2), vb_.v())
        k.barrier()
        k.release_since(m0)
    with ExitStack() as es:
        m0 = k.mark()
        wq = load_w_bf16(k, c, es, d["peer_w_q"][li], D, 2048, f"wq{li}")
        KT = k.sb([128, 2, 128], BF16, es=es)
        skf = k.sb([128, 128], F32, es=es)
        for i in range(2):
            k.dma(skf.v(), d["peer_sub_keys"][li, i])
            b = lbank()
            k.tr(b[:, 0:128], skf.v(), c.identf.v())
            k.copy("dve", KT[:, i, :], b[:, 0:128])
        gfull = k.sb([128, D], F32, es=es)
        k.dma(gfull.v(), d["ffn_norm"][li, :].pbc(128))
        if final:
            gfin = k.sb([128, D], F32, es=es)
            k.dma(gfin.v(), d["final_norm"].v().pbc(128))
        iota = k.sb([128, 128], F32, es=es)
        k.dma(iota.v(), d["c_iota"].v())
        Gbuf = k.sb([128, 128, TB], BF16, es=es)
        hnT = k.sb([128, 8, TB], BF16, es=es)
        qTall = k.sb([128, 16, TB], BF16, es=es)
        xkeep = [k.sb([128, D], F32, es=es) for _ in range(2)]
        sc = k.sb([128, 2048], F32, es=es)
        cand = k.sb([128, 8, 256], F32, es=es)
        tmpg = k.sb([128, 16, 128], F32, es=es)
        hb = k.sb([128, D], BF16, es=es)
        ss = k.sb([128, 1], F32, es=es)
        rs = k.sb([128, 1], F32, es=es)
        ts_ = k.sb([128, 8, 2, 16], F32, es=es)
        ix = k.sb([128, 8, 16], U32, es=es)
        best = k.sb([128, 8, 16], F32, es=es)
        eb = k.sb([128, 8, 16], F32, es=es)
        Z = k.sb([128, 8], F32, es=es)
        tau = k.sb([128, 8], F32, es=es)
        tok3 = [k.sb([128, 8, 16], F32, es=es) for _ in range(3)]
        jT = [k.sb([128, TB], F32, es=es) for _ in range(3)]
        ub = [k.sb([128, 2, 8, 128], BF16, es=es) for _ in range(3)]
        vbuf = [k.sb([128, 2, D], BF16, es=es) for _ in range(3)]
        oa2 = [k.sb([128, 16, 128], BF16, es=es) for _ in range(2)]
        q1r_single = k.sb([128, 16, 128], BF16, es=es)
        q1r2 = [q1r_single, q1r_single]
        e1 = [k.sb([128, 512], BF16, es=es) for _ in range(2)]
        mm_ = [k.sb([128, 512], BF16, es=es) for _ in range(2)]
        ag = [k.sb([128, TB], BF16, es=es) for _ in range(2)]
        for tb in range(NBK):
            for tl in range(2):
                t0 = tb * TB + tl * 128
                xk = xkeep[tl]
                k.dma(xk.v(), xin[t0:t0 + 128, :])
                rmsnorm_tile(k, c, xk.v(), gfull.v(), hb.v(), D, sc[:, 0:D], ss.v(), rs.v())
                b = lbank()
                pb = b.v().bitcast(BF16)
                for kk in range(8):
                    k.tr(pb[:, kk * 128:(kk + 1) * 128], hb[:, kk * 128:(kk + 1) * 128], c.identb.v())
                k.copy("act", hnT[:, :, tl * 128:(tl + 1) * 128], pb.re("p (k t) -> p k t", k=8))
            for g2 in range(8):
                b = lbank()
                for gg in range(2):
                    g = g2 * 2 + gg
                    for kk in range(8):
                        k.mm(b[:, gg * TB:(gg + 1) * TB], wq[:, kk, g * 128:(g + 1) * 128], hnT[:, kk, :], start=(kk == 0), stop=(kk == 7))
                k.copy("act" if g2 % 2 else "dve", qTall[:, g2 * 2:g2 * 2 + 2, :], b.v().re("p (g t) -> p g t", g=2))
            for tl in range(2):
                sbk = [lbank() for _ in range(4)]
                for g in range(16):
                    k.mm(sbk[g // 4][:, (g % 4) * 128:(g % 4 + 1) * 128], qTall[:, g, tl * 128:(tl + 1) * 128], KT[:, g % 2, :])
                for q4 in range(4):
                    k.copy("act" if q4 % 2 else "dve", sc[:, q4 * 512:(q4 + 1) * 512], sbk[q4].v())
                G16 = [(g, g // 2, g % 2, sc[:, g * 128:(g + 1) * 128]) for g in range(16)]
                for (g, h, i, sg) in G16:
                    k.op("dve", (lambda o=ts_[:, h, i, 0:8], x=sg: lambda e: e.max(out=o.ap, in_=x.ap))(), [sc], [ts_])
                for (g, h, i, sg) in G16:
                    k.op("dve", (lambda o=tmpg[:, g, :], m=ts_[:, h, i, 0:8], x=sg: lambda e: e.match_replace(out=o.ap, in_to_replace=m.ap, in_values=x.ap, imm_value=NEG))(), [sc, ts_], [tmpg])
                for (g, h, i, sg) in G16:
                    if i == 0:
                        k.op("dve", (lambda o=ix[:, h, 0:8], m=ts_[:, h, i, 0:8], x=sg: lambda e: e.max_index(out=o.ap, in_max=m.ap, in_values=x.ap))(), [sc, ts_], [ix])
                for (g, h, i, sg) in G16:
                    k.op("dve", (lambda o=ts_[:, h, i, 8:16], x=tmpg[:, g, :]: lambda e: e.max(out=o.ap, in_=x.ap))(), [tmpg], [ts_])
                for (g, h, i, sg) in G16:
                    if i == 0:
                        k.op("dve", (lambda o=ix[:, h, 8:16], m=ts_[:, h, i, 8:16], x=tmpg[:, g, :]: lambda e: e.max_index(out=o.ap, in_max=m.ap, in_values=x.ap))(), [tmpg, ts_], [ix])
                k.tt("pool", cand.v().re("p h (a b) -> p h a b", a=16), ts_[:, :, 0, :].us(3).bc([128, 8, 16, 16]), ts_[:, :, 1, :].us(2).bc([128, 8, 16, 16]), ALU.add)
                for h in range(8):
                    k.op("dve", (lambda o=best[:, h, 0:8], x=cand[:, h, :]: lambda e: e.max(out=o.ap, in_=x.ap))(), [cand], [best])
                for h in range(8):
                    k.op("dve", (lambda o=tmpg[:, 2 * h:2 * h + 2, :].re("p a b -> p (a b)"), m=best[:, h, 0:8], x=cand[:, h, :]: lambda e: e.match_replace(out=o.ap, in_to_replace=m.ap, in_values=x.ap, imm_value=NEG))(), [cand, best], [tmpg])
                for h in range(8):
                    k.op("dve", (lambda o=best[:, h, 8:16], x=tmpg[:, 2 * h:2 * h + 2, :].re("p a b -> p (a b)"): lambda e: e.max(out=o.ap, in_=x.ap))(), [tmpg], [best])
                thr, coef, idxf = tok3
                k.ts("dve", tau.v(), best[:, :, 15], -1e-5, None, op0=ALU.add)
                k.tt("dve", eb.v(), best.v(), best[:, :, 0:1].bc([128, 8, 16]), ALU.subtract)
                k.act(eb.v(), eb.v(), AF.Exp)
                k.op("dve", lambda e: e.tensor_reduce(out=Z.v().ap, in_=eb.v().ap, axis=AX.X, op=ALU.add), [eb], [Z])
                k.recip(Z.v(), Z.v())
                k.tt("dve", coef.v(), ts_[:, :, 0, :], best[:, :, 0:1].bc([128, 8, 16]), ALU.subtract)
                k.act(coef.v(), coef.v(), AF.Exp)
                k.tt("dve", coef.v(), coef.v(), Z.v().us(2).bc([128, 8, 16]), ALU.mult)
                k.tt("dve", thr.v(), tau.v().us(2).bc([128, 8, 16]), ts_[:, :, 0, :], ALU.subtract)
                k.copy("dve", idxf.v(), ix.v())
                for q3 in range(3):
                    b = lbank()
                    k.tr(b[:, 0:128], tok3[q3].v().re("p h a -> p (h a)"), c.identf.v())
                    k.copy("act", jT[q3][:, tl * 128:(tl + 1) * 128], b[:, 0:128])
            thrT, coefT, idxT = jT
            if c.dbg == "peer_p1":
                break
            def build16(g16):
                b0 = g16 * 16
                oa_ = oa2[g16 % 2]
                k.copy("dve", q1r2[0].v().re("p t (h r) -> p t h r", r=16),
                       qTall.v().re("p (h i) t -> p i t h", i=2)[:, 1, b0:b0 + 16, :].us(3).bc([128, 16, 8, 16]))
                k.tt("dve", oa_.v(), iota.v().us(1).bc([128, 16, 128]), idxT[:, b0:b0 + 16].us(2).bc([128, 16, 128]), ALU.is_equal)
                k.tt("dve", oa_.v(), oa_.v(), coefT[:, b0:b0 + 16].us(2).bc([128, 16, 128]), ALU.mult)
            build16(0)
            for t16 in range(TB // 16):
                t0 = t16 * 16
                oa = oa2[t16 % 2]
                q1r = q1r2[0]
                def emit_s1(t4):
                    sbk = lbank()
                    for q in range(4):
                        k.mm(sbk[:, q * 128:(q + 1) * 128], q1r[:, t4 * 4 + q, :], KT[:, 1, :])
                    return sbk
                pend = emit_s1(0)
                k.act(e1[0].v(), pend.v(), AF.Exp)
                for t4 in range(4):
                    tt0 = t0 + t4 * 4
                    sbk = pend
                    if t4 + 1 < 4:
                        pend = emit_s1(t4 + 1)
                        k.act(e1[(t4 + 1) % 2].v(), pend.v(), AF.Exp)
                    e_ = e1[t4 % 2]
                    m_ = mm_[t4 % 2]
                    k.tt("dve", m_.v().re("p (t b) -> p t b", t=4), sbk.v().re("p (t b) -> p t b", t=4), thrT[:, tt0:tt0 + 4].us(2).bc([128, 4, 128]), ALU.is_ge)
                    k.tt("dve", m_.v(), m_.v(), e_.v(), ALU.mult)
                    gbk = lbank()
                    for q in range(4):
                        k.mm(gbk[:, q * 128:(q + 1) * 128], m_[:, q * 128:(q + 1) * 128], oa[:, t4 * 4 + q, :])
                    k.copy("act", Gbuf[:, :, tt0:tt0 + 4].re("p a t -> p t a"), gbk.v().re("p (t a) -> p t a", t=4))
                    if t4 == 2 and t16 + 1 < TB // 16:
                        build16(t16 + 1)
            if c.dbg == "peer_p2":
                break
            def load_pair(a2):
                k.dma(ub[a2 % 3].v(), uT[a2 * 2:a2 * 2 + 2].re("a p k e -> p a k e"))
                k.dma(vbuf[a2 % 3].v(), vb[a2 * 256:(a2 + 1) * 256, :].re("(a p) f -> p a f", a=2))

            def emit_A(a):
                a2, ai = a // 2, a % 2
                if ai == 0 and a2 + 1 < NCH // 2:
                    load_pair(a2 + 1)
                ab = lbank()
                for kk in range(8):
                    k.mm(ab[:, 0:TB], ub[a2 % 3][:, ai, kk, :], hnT[:, kk, :], start=(kk == 0), stop=(kk == 7))
                return ab
            load_pair(0)
            pend = emit_A(0)
            for a in range(NCH):
                a2, ai = a // 2, a % 2
                ab = pend
                if a + 1 < NCH:
                    pend = emit_A(a + 1)
                g_ = ag[a % 2]
                k.act(g_.v(), ab[:, 0:TB], AF.Gelu)
                k.tt("dve", g_.v(), g_.v(), Gbuf[:, a, :], ALU.mult)
                for tl in range(2):
                    for half in range(2):
                        k.mm(c.banks[tl * 2 + half].v(), g_[:, tl * 128:(tl + 1) * 128], vbuf[a2 % 3][:, ai, half * 512:(half + 1) * 512], start=(a == 0), stop=(a == NCH - 1))
            for tl in range(2):
                t0 = tb * TB + tl * 128
                xk = xkeep[tl]
                for half in range(2):
                    k.tt("dve", xk[:, half * 512:(half + 1) * 512], xk[:, half * 512:(half + 1) * 512], c.banks[tl * 2 + half].v(), ALU.add)
                if final:
                    k.act(sc[:, 0:D], xk.v(), AF.Square, accum=ss.v())
                    k.act(rs.v(), ss.v(), AF.Sqrt, bias=c.eps.v(), scale=1.0 / D)
                    k.recip(rs.v(), rs.v())
                    k.stt("dve", xk.v(), xk.v(), rs.v(), gfin.v(), ALU.mult, ALU.mult)
                k.dma(xout[t0:t0 + 128, :], xk.v())
            k.barrier()
        k.barrier()
        k.release_since(m0)


def layer1_mixer(k, c, xin, xout):
    d = c.d
    S_ = c.S
    NT_ = S_ // 128
    with ExitStack() as es:
        m0 = k.mark()
        win = load_w_bf16(k, c, es, d["b_w_in"], D, B_IN, "b_win")
        wo = load_w_bf16(k, c, es, d["w_o"][1], D, D, "wo1")
        gfull = k.sb([128, D], F32, es=es)
        k.dma(gfull.v(), d["mix_norm"][1, :].pbc(128))
        onorm = k.sb([128, 768], F32, es=es)
        k.dma(onorm.v(), d["b_out_norm"][0, :].pbc(128))
        cw5 = k.sb([5, 1280], F32, es=es)
        k.dma(cw5[0:4, :], d["b_conv_w"].v())
        k.dma(cw5[4:5, :], d["b_conv_b"].v())
        cwb = k.sb([128, 10, 5], F32, es=es)
        b = bank(c)
        for ck in range(10):
            k.tr(b[:, ck * 8:ck * 8 + 5], cw5[:, ck * 128:(ck + 1) * 128], c.identf[0:5, 0:5])
        k.copy("dve", cwb.v(), b[:, 0:80].re("p (k e) -> p k e", e=8)[:, :, 0:5])
        dtb = k.sb([128, 12], F32, es=es)
        k.dma(dtb.v(), d["b_dt_bias"][0, :].pbc(128))
        arow = k.sb([128, 12], F32, es=es)
        k.dma(arow.v(), d["b_a_log"][0, :].pbc(128))
        k.act(arow.v(), arow.v(), AF.Exp)
        k.ts("dve", arow.v(), arow.v(), -1.0, None, op0=ALU.mult)
        dsk = k.sb([128, 12], F32, es=es)
        k.dma(dsk.v(), d["b_d_skip"][0, :].pbc(128))
        Dfull = k.sb([128, 12, 64], F32, es=es)
        k.copy("dve", Dfull.v(), dsk.v().us(2).bc([128, 12, 64]))
        triu = k.sb([128, 128], F32, es=es)
        k.dma(triu.v(), d["c_triu"].v())
        negtri = k.sb([128, 128], F32, es=es)
        k.ts("dve", negtri.v(), triu.v(), -1.0, -NEG, op0=ALU.add, op1=ALU.mult)
        ones = k.sb([128, 128], F32, es=es)
        k.memset("pool", ones.v(), 1.0)
        hst = k.sb([128, 12, 64], F32, es=es)
        hbf = k.sb([128, 12, 64], BF16, es=es)
        k.memset("pool", hst.v(), 0.0)
        k.memset("pool", hbf.v(), 0.0)
        xr = k.sb([128, 10, 131], F32, es=es)
        k.memset("pool", xr.v(), 0.0)
        xt = [k.sb([128, D], F32, es=es) for _ in range(2)]
        scr = k.sb([128, D], F32, es=es)
        hb = k.sb([128, D], BF16, es=es)
        ss = k.sb([128, 1], F32, es=es)
        rs = k.sb([128, 1], F32, es=es)
        hT = k.sb([128, 8, 128], BF16, es=es)
        acc = k.sb([128, 10, 128], F32, es=es)
        xc = k.sb([128, 10, 128], BF16, es=es)
        xtok = k.sb([128, D], BF16, es=es)
        zs = k.sb([128, 768], F32, es=es)
        dt = k.sb([128, 12], F32, es=es)
        da = k.sb([128, 12], F32, es=es)
        acs = k.sb([128, 12], F32, es=es)
        dte = k.sb([128, 12], F32, es=es)
        qm = k.sb([64, 4, 128], BF16, es=es)
        Uda = k.sb([128, 12, 128], F32, es=es)
        tmp = k.sb([128, 12, 128], F32, es=es)
        Lexp = k.sb([128, 12, 128], BF16, es=es)
        Eall = k.sb([128, 12, 128], F32, es=es)
        cbt = k.sb([128, 2, 128], BF16, es=es)
        MTm = k.sb([128, 12, 128], BF16, es=es)
        CTs = k.sb([128, 12, 128], BF16, es=es)
        Xd = k.sb([128, 12, 64], BF16, es=es)
        Xdd = k.sb([128, 12, 64], BF16, es=es)
        yv = k.sb([128, 768], F32, es=es)
        cat = k.sb([128, 1, D], BF16, es=es)
        wk = {"pt": [k.sb([128, 512], BF16, es=es) for _ in range(2)], "rden": k.sb([128, 4], F32, es=es),
              "catT": k.sb([128, D], BF16, es=es), "xres": [k.sb([128, D], F32, es=es) for _ in range(2)], "xi": 0}
        for j in range(NT_):
            x_ = xt[j % 2]
            k.dma(x_.v(), xin[j * 128:(j + 1) * 128, :])
            rmsnorm_tile(k, c, x_.v(), gfull.v(), hb.v(), D, scr.v(), ss.v(), rs.v())
            b = bank(c)
            pb = b.v().bitcast(BF16)
            for kk in range(8):
                k.tr(pb[:, kk * 128:(kk + 1) * 128], hb[:, kk * 128:(kk + 1) * 128], c.identb.v())
            k.copy("act", hT.v(), pb.re("p (k t) -> p k t", k=8))
            for c0 in range(0, 10, 4):
                n_ = min(4, 10 - c0)
                b = bank(c)
                for ci in range(n_):
                    ck = c0 + ci
                    for kk in range(8):
                        k.mm(b[:, ci * 128:(ci + 1) * 128], win[:, kk, 768 + ck * 128:768 + (ck + 1) * 128], hT[:, kk, :], start=(kk == 0), stop=(kk == 7))
                k.copy("act", xr[:, c0:c0 + n_, 3:131], b[:, 0:n_ * 128].re("p (k t) -> p k t", k=n_))
            for ck in range(10):
                k.ts("dve", acc[:, ck, :], xr[:, ck, 0:128], cwb[:, ck, 0:1], cwb[:, ck, 4:5], op0=ALU.mult, op1=ALU.add)
                for jj in range(1, 4):
                    k.stt("dve", acc[:, ck, :], xr[:, ck, jj:jj + 128], cwb[:, ck, jj:jj + 1], acc[:, ck, :], ALU.mult, ALU.add)
            k.copy("pool", xr[:, :, 0:3], xr[:, :, 128:131])
            k.act(xc.v(), acc.v(), AF.Silu)
            b = bank(c)
            pb = b.v().bitcast(BF16)
            for ck in range(8):
                k.tr(pb[:, ck * 128:(ck + 1) * 128], xc[:, ck, :], c.identb.v())
            k.copy("act", xtok.v(), pb)
            b0 = bank(c)
            b1 = bank(c)
            for kk in range(8):
                k.mm(b0.v(), hT[:, kk, :], win[:, kk, 0:512], start=(kk == 0), stop=(kk == 7))
            for kk in range(8):
                k.mm(b1[:, 0:256], hT[:, kk, :], win[:, kk, 512:768], start=(kk == 0), stop=(kk == 7))
            k.act(zs[:, 0:512], b0.v(), AF.Silu)
            k.act(zs[:, 512:768], b1[:, 0:256], AF.Silu)
            b = bank(c)
            for kk in range(8):
                k.mm(b[:, 0:12], hT[:, kk, :], win[:, kk, 2048:2060], start=(kk == 0), stop=(kk == 7))
            k.tt("dve", dt.v(), b[:, 0:12], dtb.v(), ALU.add)
            k.act(dt.v(), dt.v(), AF.Exp)
            k.act(dt.v(), dt.v(), AF.Ln, bias=c.one.v())
            k.tt("dve", da.v(), dt.v(), arow.v(), ALU.mult)
            b = bank(c)
            for h in range(4):
                for kk in range(8):
                    k.mm(b[0:64, h * 128:(h + 1) * 128], win[:, kk, 2060 + h * 64:2060 + (h + 1) * 64], hT[:, kk, :], start=(kk == 0), stop=(kk == 7))
            k.copy("act", qm.v(), b[0:64, :].re("p (h t) -> p h t", h=4))
            b = bank(c)
            k.mm(b[:, 0:12], triu.v(), da.v())
            k.copy("dve", acs.v(), b[:, 0:12])
            k.tt("dve", Uda.v(), triu.v().us(1).bc([128, 12, 128]), da.v().us(2).bc([128, 12, 128]), ALU.mult)
            arow_ps = psv(c, 0, 3)
            for q in range(3):
                k.mm(c.banks[q].v(), ones.v(), Uda[:, q * 4:(q + 1) * 4, :].re("p h l -> p (h l)"))
            ar3 = arow_ps.re("p (h l) -> p h l", h=12)
            k.tt("dve", tmp.v(), ar3, acs.v().us(2).bc([128, 12, 128]), ALU.subtract)
            k.tt("pool", tmp.v(), tmp.v(), negtri.v().us(1).bc([128, 12, 128]), ALU.add)
            k.act(Lexp.v(), tmp.v(), AF.Exp)
            k.act(Eall.v(), ar3, AF.Exp)
            k.tt("dve", dte.v(), ar3[:, :, 127], acs.v(), ALU.subtract)
            k.act(dte.v(), dte.v(), AF.Exp)
            b = bank(c)
            for g in range(2):
                k.mm(b[:, g * 128:(g + 1) * 128], xc[:, 6 + g, :], xc[:, 8 + g, :])
            k.copy("act", cbt.v(), b[:, 0:256].re("p (g l) -> p g l", g=2))
            k.tt("dve", MTm.v().re("p (g r) l -> p g r l", g=2), Lexp.v().re("p (g r) l -> p g r l", g=2), cbt.v().us(2).bc([128, 2, 6, 128]), ALU.mult)
            k.tt("pool", Xd.v(), xtok[:, 0:768].re("p (r e) -> p r e", e=64), dt.v().us(2).bc([128, 12, 64]), ALU.mult)
            k.tt("dve", CTs.v().re("p (g r) l -> p g r l", g=2), Eall.v().re("p (g r) l -> p g r l", g=2), xc[:, 8:10, :].us(2).bc([128, 2, 6, 128]), ALU.mult)
            k.tt("pool", Xdd.v(), Xd.v(), dte.v().us(2).bc([128, 12, 64]), ALU.mult)
            ybA = bank(c)
            ybB = bank(c)
            for r in range(12):
                yb = ybA if r < 8 else ybB
                cs_ = (r % 8) * 64
                k.mm(yb[:, cs_:cs_ + 64], MTm[:, r, :], Xd[:, r, :], start=(r % 8 == 0), stop=False)
                k.mm(yb[:, cs_:cs_ + 64], CTs[:, r, :], hbf[:, r, :], start=False, stop=True)
            sbA = bank(c)
            sbB = bank(c)
            for r in range(12):
                sbk = sbA if r < 8 else sbB
                cs_ = (r % 8) * 64
                g = r // 6
                k.mm(sbk[:, cs_:cs_ + 64], xtok[:, 768 + g * 128:768 + (g + 1) * 128], Xdd[:, r, :], start=(r % 8 == 0), stop=True)
            k.tt("pool", yv.v(), xtok[:, 0:768], Dfull.v().re("p r e -> p (r e)"), ALU.mult)
            k.tt("dve", yv[:, 0:512], yv[:, 0:512], ybA.v(), ALU.add)
            k.tt("dve", yv[:, 512:768], yv[:, 512:768], ybB[:, 0:256], ALU.add)
            k.tt("dve", yv.v(), yv.v(), zs.v(), ALU.mult)
            k.act(scr[:, 0:768], yv.v(), AF.Square, accum=ss.v())
            k.act(rs.v(), ss.v(), AF.Sqrt, bias=c.eps.v(), scale=1.0 / 768)
            k.recip(rs.v(), rs.v())
            k.stt("dve", cat[:, 0, 0:768], yv.v(), rs.v(), onorm.v(), ALU.mult, ALU.mult)
            k.tt("pool", hst.v(), hst.v(), Eall[:, :, 127:128].bc([128, 12, 64]), ALU.mult)
            k.tt("dve", hst[:, 0:8, :], hst[:, 0:8, :], sbA.v().re("p (r e) -> p r e", e=64), ALU.add)
            k.tt("dve", hst[:, 8:12, :], hst[:, 8:12, :], sbB[:, 0:256].re("p (r e) -> p r e", e=64), ALU.add)
            k.copy("act", hbf.v(), hst.v())
            mem_attention(k, c, 1, qm.v(), 1, cat.v(), wk)
            out_proj_residual(k, c, cat[:, 0, :], wo, xin, xout, j * 128, wk)
        k.barrier()
        k.release_since(m0)

def build_program(S_, stages, dbg=None):
    nc = bass.Bass("TRN2", target_bir_lowering=False)
    c = Ctx()
    c.dbg = dbg
    c.S = S_
    c.topk = min(256, S_ // 4)
    es = ExitStack()
    with es:
        k = K(nc, es)
        c.d = {}

        def inp(name, shape, dt=F32):
            c.d[name] = k.reg(Buf(nc.dram_tensor(name, list(shape), dt, kind="ExternalInput"), name, "dr"))

        inp("x", [S_, D])
        inp("mem", [256, D])
        inp("mem_norm", [D])
        inp("rel_bias", [32, 12])
        inp("mix_norm", [2, D])
        inp("ffn_norm", [2, D])
        inp("final_norm", [D])
        inp("w_o", [2, D, D])
        inp("w_mem_kv", [2, D, 512])
        inp("a_w_in", [D, A_IN])
        inp("a_kv_norm", [1, 256])
        inp("a_w_uk", [12, 256, 64])
        inp("a_w_uv", [12, 256, 64])
        inp("b_w_in", [D, B_IN])
        inp("b_conv_w", [4, 1280])
        inp("b_conv_b", [1, 1280])
        inp("b_dt_bias", [1, 12])
        inp("b_a_log", [1, 12])
        inp("b_d_skip", [1, 12])
        inp("b_out_norm", [1, 768])
        inp("peer_w_q", [2, D, 2048])
        inp("peer_sub_keys", [2, 2, 128, 128])
        inp("peer_u", [2, 16384, D])
        inp("peer_v", [2, 16384, D])
        inp("c_ident", [128, 128])
        inp("c_cbias", [128, 128])
        inp("c_ohr", [32, 512])
        inp("c_oh31", [32, 128])
        inp("c_iota", [128, 128])
        inp("c_triu", [128, 128])
        out = k.reg(Buf(nc.dram_tensor("out", [S_, D], F32, kind="ExternalOutput"), "out", "dr"))
        setup_common(k, c)
        phase_mem(k, c)
        x1 = out if stages == 1 else k.dram("x1", [S_, D], F32)
        if stages == 2:
            peer_layer(k, c, 0, c.d["x"], out, False)
        elif stages == 5:
            layer1_mixer(k, c, c.d["x"], out)
        else:
            layer0_mixer(k, c, c.d["x"], x1)
        if stages == 3:
            layer1_mixer(k, c, x1, out)
        elif stages == 99:
            x2 = k.dram("x2", [S_, D], F32)
            peer_layer(k, c, 0, x1, x2, False)
            x3 = k.dram("x3", [S_, D], F32)
            layer1_mixer(k, c, x2, x3)
            peer_layer(k, c, 1, x3, out, True)
        k.barrier()
        global LASTK
        LASTK = k
        with nc.Block() as block:
            k.emit(block)
    return nc


def t5_bucket_np(dist):
    n = np.maximum(dist, 0)
    me = 16
    nf = np.maximum(n, me).astype(np.float32)
    large = me + (np.log(nf / me) / math.log(128 / me) * (32 - me)).astype(np.int32)
    large = np.minimum(large, 31)
    return np.where(n < me, n, large)


def host_consts():
    cs = {}
    cs["c_ident"] = np.eye(128, dtype=np.float32)
    tt = np.arange(128)
    cs["c_cbias"] = np.where(tt[None, :] <= tt[:, None], 0.0, NEG).astype(np.float32)
    ohr = np.zeros((32, 512), np.float32)
    for m in range(256):
        ohr[t5_bucket_np(np.array(255 - m)), m] += 1.0
        ohr[31, m] -= 1.0
    cs["c_ohr"] = ohr
    oh31 = np.zeros((32, 128), np.float32)
    oh31[31, :] = 1.0
    cs["c_oh31"] = oh31
    cs["c_iota"] = np.tile(np.arange(128, dtype=np.float32)[None, :], (128, 1))
    cs["c_triu"] = (tt[:, None] <= tt[None, :]).astype(np.float32)
    return cs


def make_in_maps(inputs, n_cores, S_):
    cs = host_consts()
    maps = []
    for b in range(n_cores):
        m = {}
        m["x"] = np.ascontiguousarray(inputs["x"][b, :S_])
        m["mem"] = np.ascontiguousarray(inputs["mem"][b])
        for nm in ("mem_norm", "rel_bias", "mix_norm", "ffn_norm", "final_norm", "w_o", "w_mem_kv",
                   "a_kv_norm", "b_conv_b", "b_dt_bias", "b_a_log", "b_d_skip", "b_out_norm",
                   "peer_w_q", "peer_sub_keys", "peer_u", "peer_v"):
            m[nm] = np.ascontiguousarray(inputs[nm])
        m["a_w_in"] = np.ascontiguousarray(inputs["a_w_in"][0])
        m["a_w_uk"] = np.ascontiguousarray(inputs["a_w_uk"][0])
        m["a_w_uv"] = np.ascontiguousarray(inputs["a_w_uv"][0])
        m["b_w_in"] = np.ascontiguousarray(inputs["b_w_in"][0])
        m["b_conv_w"] = np.ascontiguousarray(inputs["b_conv_w"][0])
        m.update(cs)
        maps.append(m)
    return maps


def kernel(**inputs):
    inputs = {k_: np.asarray(v) for k_, v in inputs.items()}
    nc = build_program(S, stages=99)
    maps = make_in_maps(inputs, 8, S)
    res = run_bass_kernel_spmd(nc, maps, core_ids=list(range(8)))
    return np.stack([r["out"] for r in res.results], axis=0).astype(np.float32)
```

```python
import math
import numpy as np
from contextlib import ExitStack
import ml_dtypes
import concourse.bass as bass
import concourse.mybir as mybir
from concourse.bass_utils import run_bass_kernel_spmd

F32 = mybir.dt.float32
BF16 = mybir.dt.bfloat16
U32 = mybir.dt.uint32
AF = mybir.ActivationFunctionType
ALU = mybir.AluOpType
AX = mybir.AxisListType

ENGS = ("pe", "act", "dve", "pool", "sp")
NEG = -1.0e30

S = 4096
D = 1024
NT = S // 128
A_IN = 1864
B_IN = 2316


class V:
    def __init__(self, bufs, ap):
        self.bufs = bufs if isinstance(bufs, list) else [bufs]
        self.ap = ap

    def __getitem__(self, idx):
        return V(self.bufs, self.ap[idx])

    def bc(self, shape):
        return V(self.bufs, self.ap.broadcast_to(list(shape)))

    def us(self, axis):
        return V(self.bufs, self.ap.unsqueeze(axis))

    def re(self, pat, **kw):
        return V(self.bufs, self.ap.rearrange(pat, **kw))

    def bitcast(self, dt):
        return V(self.bufs, self.ap.bitcast(dt))

    def pbc(self, n):
        return V(self.bufs, self.ap.partition_broadcast(n))


class Buf:
    def __init__(self, t, name, space, view=None):
        self.t = t
        self.name = name
        self.space = space
        self.view = view if view is not None else (t.ap() if space == "dr" else t[:])
        self.w = []
        self.r = []
        self.dsem = None
        self.dcnt = 0

    def __getitem__(self, idx):
        return V(self, self.view[idx])

    def v(self):
        return V(self, self.view)


class K:
    def __init__(self, nc, es):
        self.nc = nc
        self.es = es
        self.ops = {e: [] for e in ENGS}
        self.sem = {e: es.enter_context(nc.semaphore("s_" + e)) for e in ENGS}
        self.cnt = {e: 0 for e in ENGS}
        self.waited = {e: {} for e in ENGS}
        self.dsems = []
        self.nbuf = 0
        self.free_dsems = []
        self.q = 0
        self.allbufs = []
        self.log = []

    def sb(self, shape, dtype, es=None, name=None):
        self.nbuf += 1
        name = name or f"sb{self.nbuf}"
        t = (es or self.es).enter_context(self.nc.sbuf_tensor(name, list(shape), dtype))
        b = Buf(t, name, "sb")
        self.allbufs.append(b)
        self.log.append(b)
        return b

    def reg(self, b):
        self.allbufs.append(b)
        return b

    def mark(self):
        return len(self.log)

    def release_since(self, m):
        self.release(self.log[m:])
        del self.log[m:]

    def dram(self, name, shape, dtype, kind="Internal"):
        t = self.nc.dram_tensor(name, list(shape), dtype, kind=kind)
        return self.reg(Buf(t, name, "dr"))

    def _dsem(self, b):
        if b.dsem is None:
            if self.free_dsems:
                b.dsem, b.dbase = self.free_dsems.pop()
            else:
                b.dsem = self.es.enter_context(self.nc.semaphore(f"d{len(self.dsems)}"))
                b.dbase = 0
            self.dsems.append(b)
        return b.dsem

    def release(self, bufs):
        for b in bufs:
            if b.dsem is not None:
                self.free_dsems.append((b.dsem, b.dbase + 16 * b.dcnt))
                self.dsems.remove(b)
                b.dsem = None

    def _need(self, eng, ev, waits, same_ok):
        if ev[0] == "E":
            _, e2, tk = ev
            if e2 == eng and same_ok:
                return
            key = ("E", e2)
            val = tk
            sem = self.sem[e2]
        else:
            owner = ev[1]
            if owner.dsem is None:
                return
            key = ("D", id(owner.dsem))
            val = owner.dbase + 16 * owner.dcnt
            sem = owner.dsem
        if self.waited[eng].get(key, 0) >= val:
            return
        self.waited[eng][key] = val
        waits.append((sem, val))

    def _deps(self, eng, reads, writes):
        waits = []
        for b in reads:
            for ev in b.w:
                self._need(eng, ev, waits, same_ok=(eng == "pe"))
        for b in writes:
            for ev in b.w:
                self._need(eng, ev, waits, same_ok=True)
            for ev in b.r:
                self._need(eng, ev, waits, same_ok=True)
        return waits

    def op(self, eng, fn, reads=(), writes=()):
        reads = list(dict.fromkeys(reads))
        writes = list(dict.fromkeys(writes))
        waits = self._deps(eng, reads, writes)
        self.cnt[eng] += 1
        ev = ("E", eng, self.cnt[eng])
        self.ops[eng].append((waits, fn, self.sem[eng], 1))
        for b in reads:
            b.r = [x for x in b.r if not (x[0] == "E" and x[1] == eng)] + [ev]
        for b in writes:
            b.w = [ev]
            b.r = []

    def dma(self, dst, src, q=None, **kw):
        if q is None:
            q = ("sp", "pool")[self.q % 2] if False else "sp"
        db, sbf = dst.bufs[0], src.bufs[0]
        owner = db if db.space != "dr" else sbf
        sem = self._dsem(owner)
        waits = self._deps(q, [sbf], [db])
        owner.dcnt += 1
        ev = ("D", owner)
        dap, sap = dst.ap, src.ap
        self.ops[q].append((waits, lambda e: e.dma_start(out=dap, in_=sap, **kw), sem, 16))
        if not any(x[0] == "D" and x[1] is owner for x in sbf.r):
            sbf.r.append(ev)
        if db.space == "dr":
            if not any(x[0] == "D" and x[1] is owner for x in db.w):
                db.w.append(ev)
        else:
            db.w = [ev]
        db.r = []

    def barrier(self, reset=()):
        for e in ENGS:
            waits = []
            for e2 in ENGS:
                if e2 != e and self.cnt[e2] > 0:
                    self._need(e, ("E", e2, self.cnt[e2]), waits, same_ok=False)
            for b in self.dsems:
                if b.dcnt:
                    self._need(e, ("D", b), waits, same_ok=False)
            if waits:
                self.ops[e].append((waits, None, None, 0))
        for b in self.allbufs:
            b.w = []
            b.r = []
        for e in ENGS:
            if self.cnt[e] > 30000:
                self.sem[e] = self.es.enter_context(self.nc.semaphore(f"s_{e}_{len(self.ops[e])}"))
                self.cnt[e] = 0
                for e2 in ENGS:
                    self.waited[e2].pop(("E", e), None)

    def emit(self, block):
        ops = self.ops

        def run(e, lst, attach):
            for waits, fn, sem, inc in lst:
                if fn is None or not attach:
                    for s, v in waits:
                        e.wait_ge(s, v)
                    if fn is not None:
                        fn(e).then_inc(sem, inc)
                    continue
                for s, v in waits[1:]:
                    e.wait_ge(s, v)
                ins = fn(e)
                if waits:
                    ins._wait_ge(waits[0][0], waits[0][1])
                ins.then_inc(sem, inc)

        @block.tensor
        def _(e):
            run(e, ops["pe"], True)

        @block.scalar
        def _(e):
            run(e, ops["act"], True)

        @block.vector
        def _(e):
            run(e, ops["dve"], True)

        @block.gpsimd
        def _(e):
            run(e, ops["pool"], True)

        @block.sync
        def _(e):
            run(e, ops["sp"], True)

    @staticmethod
    def _rw(outs, ins):
        r, w = [], []
        for x in ins:
            if isinstance(x, V):
                r += x.bufs
        for x in outs:
            if isinstance(x, V):
                w += x.bufs
        return r, w

    @staticmethod
    def _a(x):
        return x.ap if isinstance(x, V) else x

    def act(self, out, in_, func, bias=None, scale=1.0, accum=None):
        r, w = self._rw([out, accum], [in_, bias, scale])
        kw = {}
        if bias is not None:
            kw["bias"] = self._a(bias)
        if accum is not None:
            kw["accum_out"] = accum.ap
        sc = self._a(scale)
        self.op("act", lambda e: e.activation(out=out.ap, in_=in_.ap, func=func, scale=sc, **kw), r, w)

    def tt(self, eng, out, in0, in1, op):
        r, w = self._rw([out], [in0, in1])
        self.op(eng, lambda e: e.tensor_tensor(out=out.ap, in0=in0.ap, in1=in1.ap, op=op), r, w)

    def ts(self, eng, out, in0, s1, s2=None, op0=ALU.mult, op1=None, accum=None):
        r, w = self._rw([out, accum], [in0, s1, s2])
        kw = {}
        if op1 is not None:
            kw["op1"] = op1
        if accum is not None:
            kw["accum_out"] = accum.ap
        a1, a2 = self._a(s1), self._a(s2)
        self.op(eng, lambda e: e.tensor_scalar(out=out.ap, in0=in0.ap, scalar1=a1, scalar2=a2, op0=op0, **kw), r, w)

    def stt(self, eng, out, in0, scalar, in1, op0, op1):
        r, w = self._rw([out], [in0, scalar, in1])
        sc = self._a(scalar)
        self.op(eng, lambda e: e.scalar_tensor_tensor(out=out.ap, in0=in0.ap, scalar=sc, in1=in1.ap, op0=op0, op1=op1), r, w)

    def copy(self, eng, out, in_):
        r, w = self._rw([out], [in_])
        if eng == "act":
            self.op(eng, lambda e: e.activation(out=out.ap, in_=in_.ap, func=AF.Copy), r, w)
        else:
            self.op(eng, lambda e: e.tensor_copy(out=out.ap, in_=in_.ap), r, w)

    def memset(self, eng, out, val):
        self.op(eng, lambda e: e.memset(out.ap, val), [], out.bufs)

    def mm(self, out, lhsT, rhs, start=True, stop=True):
        r, w = self._rw([out], [lhsT, rhs])
        self.op("pe", lambda e: e.matmul(out.ap, lhsT=lhsT.ap, rhs=rhs.ap, start=start, stop=stop, skip_group_check=True), r, w)

    def tr(self, out, in_, ident):
        r, w = self._rw([out], [in_, ident])
        self.op("pe", lambda e: e.transpose(out=out.ap, in_=in_.ap, identity=ident.ap), r, w)

    def recip(self, out, in_):
        r, w = self._rw([out], [in_])
        self.op("dve", lambda e: e.reciprocal(out=out.ap, in_=in_.ap), r, w)


class Ctx:
    pass


def setup_common(k, c):
    nc = k.nc
    pst = k.es.enter_context(nc.psum_tensor("psall", [128, 4096], F32))
    c.psall = pst
    c.banks = [k.reg(Buf(pst, f"bank{b}", "ps", view=pst[:, b * 512:(b + 1) * 512])) for b in range(8)]
    c.nb = 0
    c.identf = k.sb([128, 128], F32)
    c.identb = k.sb([128, 128], BF16)
    k.dma(c.identf.v(), c.d["c_ident"].v())
    k.copy("dve", c.identb.v(), c.identf.v())
    c.eps = k.sb([128, 1], F32)
    k.memset("pool", c.eps.v(), 1e-6)
    c.one = k.sb([128, 1], F32)
    k.memset("pool", c.one.v(), 1.0)


def bank(c):
    b = c.banks[c.nb % 8]
    c.nb += 1
    return b


def psv(c, b0, n):
    return V([c.banks[b0 + i] for i in range(n)], c.psall[:, b0 * 512:(b0 + n) * 512])


def rmsnorm_tile(k, c, xin, gfull, out_bf, width, scr, ss, rs):
    k.act(scr, xin, AF.Square, accum=ss)
    k.act(rs, ss, AF.Sqrt, bias=c.eps.v(), scale=1.0 / width)
    k.recip(rs, rs)
    k.stt("dve", out_bf, xin, rs, gfull, ALU.mult, ALU.mult)


def load_w_bf16(k, c, es, dsrc, rows, cols, name, col0=0, ncols=None, stage=None):
    ncols = ncols or cols
    kc = rows // 128
    wt = k.sb([128, kc, ncols], BF16, es=es, name=name)
    with ExitStack() as es2:
        st = [k.sb([128, min(ncols, 2048)], F32, es=es2) for _ in range(2)]
        i = 0
        for kk in range(kc):
            for c0 in range(0, ncols, 2048):
                w_ = min(2048, ncols - c0)
                s = st[i % 2]
                i += 1
                k.dma(s[:, 0:w_], dsrc[kk * 128:(kk + 1) * 128, col0 + c0:col0 + c0 + w_])
                k.copy("pool" if i % 2 else "dve", wt[:, kk, c0:c0 + w_], s[:, 0:w_])
        k.barrier()
        k.release(st)
    return wt


def phase_mem(k, c):
    d = c.d
    c.kmT = [k.sb([64, 4, 256], BF16) for _ in range(2)]
    c.vm = [k.sb([128, 2, 4, 65], BF16) for _ in range(2)]
    with ExitStack() as es:
        gfull = k.sb([128, D], F32, es=es)
        k.dma(gfull.v(), d["mem_norm"].v().pbc(128))
        mnT = k.sb([128, 8, 256], BF16, es=es)
        xt = k.sb([128, D], F32, es=es)
        scr = k.sb([128, D], F32, es=es)
        hb = k.sb([128, D], BF16, es=es)
        ss = k.sb([128, 1], F32, es=es)
        rs = k.sb([128, 1], F32, es=es)
        for mt in range(2):
            k.dma(xt.v(), d["mem"][mt * 128:(mt + 1) * 128, :])
            rmsnorm_tile(k, c, xt.v(), gfull.v(), hb.v(), D, scr.v(), ss.v(), rs.v())
            b = bank(c)
            pb = b.v().bitcast(BF16)
            for kk in range(8):
                k.tr(pb[:, kk * 128:(kk + 1) * 128], hb[:, kk * 128:(kk + 1) * 128], c.identb.v())
            k.copy("dve", mnT[:, :, mt * 128:(mt + 1) * 128], pb.re("p (k t) -> p k t", k=8))
        for i in range(2):
            wkv = load_w_bf16(k, c, es, d["w_mem_kv"][i], D, 512, f"wkv{i}")
            for h in range(4):
                b = bank(c)
                for kk in range(8):
                    k.mm(b[0:64, 0:256], wkv[:, kk, h * 64:(h + 1) * 64], mnT[:, kk, :], start=(kk == 0), stop=(kk == 7))
                k.copy("act", c.kmT[i][:, h, :], b[0:64, 0:256])
            k.memset("pool", c.vm[i].v(), 1.0)
            for mt in range(2):
                b = bank(c)
                for kk in range(8):
                    k.mm(b[:, 0:256], mnT[:, kk, mt * 128:(mt + 1) * 128], wkv[:, kk, 256:512], start=(kk == 0), stop=(kk == 7))
                k.copy("act", c.vm[i][:, mt, :, 0:64], b[:, 0:256].re("p (h e) -> p h e", h=4))
        k.barrier()


def mem_attention(k, c, li, qm, nq, cat, wk):
    pt = wk["pt"]
    for h in range(4):
        ob = bank(c)
        for mt in range(2):
            sb_ = bank(c)
            k.mm(sb_[:, 0:nq * 128], c.kmT[li][:, h, mt * 128:(mt + 1) * 128], qm[:, h, :])
            p = pt[mt % 2]
            k.act(p[:, 0:nq * 128], sb_[:, 0:nq * 128], AF.Exp, scale=0.125)
            for jl in range(nq):
                k.mm(ob[:, jl * 65:(jl + 1) * 65], p[:, jl * 128:(jl + 1) * 128], c.vm[li][:, mt, h, :], start=(mt == 0 and jl == 0), stop=(mt == 1))
        rd = wk["rden"]
        ov = ob[:, 0:nq * 65].re("p (j e) -> p j e", e=65)
        k.recip(rd[:, 0:nq], ov[:, :, 64])
        k.tt("dve", cat[:, 0:nq, 768 + h * 64:768 + (h + 1) * 64], ov[:, :, 0:64], rd[:, 0:nq].us(2).bc([128, nq, 64]), ALU.mult)


def out_proj_residual(k, c, cat_t, wo, xin_dram, xout_dram, t0, wk):
    b = bank(c)
    pb = b.v().bitcast(BF16)
    for kk in range(8):
        k.tr(pb[:, kk * 128:(kk + 1) * 128], cat_t[:, kk * 128:(kk + 1) * 128], c.identb.v())
    catT = wk["catT"]
    k.copy("act", catT.v(), pb)
    xt = wk["xres"][wk["xi"] % 2]
    wk["xi"] += 1
    k.dma(xt.v(), xin_dram[t0:t0 + 128, :])
    for half in range(2):
        ob = bank(c)
        for kk in range(8):
            k.mm(ob.v(), catT[:, kk * 128:(kk + 1) * 128], wo[:, kk, half * 512:(half + 1) * 512], start=(kk == 0), stop=(kk == 7))
        k.tt("dve", xt[:, half * 512:(half + 1) * 512], xt[:, half * 512:(half + 1) * 512], ob.v(), ALU.add)
    k.dma(xout_dram[t0:t0 + 128, :], xt.v())


def layer0_mixer(k, c, xin, xout):
    d = c.d
    nc = k.nc
    S_ = c.S
    NT_ = S_ // 128
    NB = S_ // 512
    qT = k.dram("qT", [12, 64, S_], BF16)
    kT = k.dram("kT", [12, 64, S_], BF16)
    iqT = k.dram("iqT", [8, 64, S_], BF16)
    qmT = k.dram("qmT", [4, 64, S_], BF16)
    MT = k.dram("MT", [NT_, 128, NT_ * 128], BF16)
    with ExitStack() as esL:
        Vres = k.sb([128, NT_, 12, 65], BF16, es=esL)
        ikT = k.sb([64, S_], BF16, es=esL)
        absw = k.sb([128, NT_, 8], F32, es=esL)
        sgnw = k.sb([128, NT_, 8], F32, es=esL)
        k.memset("pool", Vres.v(), 1.0)
        with ExitStack() as es:
            win = load_w_bf16(k, c, es, d["a_w_in"], D, A_IN, "a_win")
            wuk = k.sb([128, 2, 12, 64], BF16, es=es)
            wuv = k.sb([128, 2, 12, 64], BF16, es=es)
            with ExitStack() as es2:
                st = k.sb([128, 2, 12, 64], F32, es=es2)
                for (dst, nm) in ((wuk, "a_w_uk"), (wuv, "a_w_uv")):
                    for cc in range(2):
                        k.dma(st[:, cc, :, :], d[nm][:, cc * 128:(cc + 1) * 128, :].re("h c e -> c h e"))
                    k.copy("dve", dst.v(), st.v())
                k.barrier()
                k.release([st])
            gfull = k.sb([128, D], F32, es=es)
            k.dma(gfull.v(), d["mix_norm"][0, :].pbc(128))
            gkv = k.sb([128, 256], F32, es=es)
            k.dma(gkv.v(), d["a_kv_norm"][0, :].pbc(128))
            xt = [k.sb([128, D], F32, es=es) for _ in range(2)]
            scr = k.sb([128, D], F32, es=es)
            hb = k.sb([128, D], BF16, es=es)
            ss = k.sb([128, 1], F32, es=es)
            rs = k.sb([128, 1], F32, es=es)
            hT = k.sb([128, 8, 512], BF16, es=es)
            cT = k.sb([128, 2, 512], BF16, es=es)
            ckv = k.sb([128, 256], F32, es=es)
            cn = k.sb([128, 256], BF16, es=es)
            iwt = k.sb([128, 8], F32, es=es)
            stg = [k.sb([64, 512], BF16, es=es) for _ in range(4)]
            si = 0
            for blk in range(NB):
                for tl in range(4):
                    j = blk * 4 + tl
                    x_ = xt[j % 2]
                    k.dma(x_.v(), xin[j * 128:(j + 1) * 128, :])
                    rmsnorm_tile(k, c, x_.v(), gfull.v(), hb.v(), D, scr.v(), ss.v(), rs.v())
                    b = bank(c)
                    pb = b.v().bitcast(BF16)
                    for kk in range(8):
                        k.tr(pb[:, kk * 128:(kk + 1) * 128], hb[:, kk * 128:(kk + 1) * 128], c.identb.v())
                    k.copy("act", hT[:, :, tl * 128:(tl + 1) * 128], pb.re("p (k t) -> p k t", k=8))
                    b = bank(c)
                    for kk in range(8):
                        k.mm(b[:, 0:256], hT[:, kk, tl * 128:(tl + 1) * 128], win[:, kk, 768:1024], start=(kk == 0), stop=(kk == 7))
                    for kk in range(8):
                        k.mm(b[:, 256:264], hT[:, kk, tl * 128:(tl + 1) * 128], win[:, kk, 1600:1608], start=(kk == 0), stop=(kk == 7))
                    k.copy("act", ckv.v(), b[:, 0:256])
                    k.copy("dve", iwt.v(), b[:, 256:264])
                    k.act(sgnw[:, j, :], iwt.v(), AF.Sign)
                    k.act(absw[:, j, :], iwt.v(), AF.Abs, scale=(8 ** -0.5) / 8.0)
                    k.act(scr[:, 0:256], ckv.v(), AF.Square, accum=ss.v())
                    k.act(rs.v(), ss.v(), AF.Sqrt, bias=c.eps.v(), scale=1.0 / 256)
                    k.recip(rs.v(), rs.v())
                    k.stt("dve", cn.v(), ckv.v(), rs.v(), gkv.v(), ALU.mult, ALU.mult)
                    b = bank(c)
                    pb = b.v().bitcast(BF16)
                    for cc in range(2):
                        k.tr(pb[:, cc * 128:(cc + 1) * 128], cn[:, cc * 128:(cc + 1) * 128], c.identb.v())
                    k.copy("act", cT[:, :, tl * 128:(tl + 1) * 128], pb[:, 0:256].re("p (k t) -> p k t", k=2))
                    b0 = bank(c)
                    b1 = bank(c)
                    for cc in range(2):
                        k.mm(b0.v(), cT[:, cc, tl * 128:(tl + 1) * 128], wuv[:, cc, 0:8, :].re("p h e -> p (h e)"), start=(cc == 0), stop=(cc == 1))
                    for cc in range(2):
                        k.mm(b1[:, 0:256], cT[:, cc, tl * 128:(tl + 1) * 128], wuv[:, cc, 8:12, :].re("p h e -> p (h e)"), start=(cc == 0), stop=(cc == 1))
                    k.copy("act", Vres[:, j, 0:8, 0:64], b0.v().re("p (h e) -> p h e", h=8))
                    k.copy("dve", Vres[:, j, 8:12, 0:64], b1[:, 0:256].re("p (h e) -> p h e", h=4))
                outs = []
                for h in range(12):
                    outs.append((h * 64, qT[h]))
                for h in range(8):
                    outs.append((1024 + h * 64, iqT[h]))
                outs.append((1536, None))
                for h in range(4):
                    outs.append((1608 + h * 64, qmT[h]))
                for (c0, dst) in outs:
                    b = bank(c)
                    for kk in range(8):
                        k.mm(b[0:64, :], win[:, kk, c0:c0 + 64], hT[:, kk, :], start=(kk == 0), stop=(kk == 7))
                    if dst is None:
                        k.copy("act", ikT[:, blk * 512:(blk + 1) * 512], b[0:64, :])
                    else:
                        s_ = stg[si % 4]
                        si += 1
                        k.copy("act" if si % 2 else "dve", s_.v(), b[0:64, :])
                        k.dma(dst[:, blk * 512:(blk + 1) * 512], s_.v())
                for h in range(12):
                    b = bank(c)
                    for cc in range(2):
                        k.mm(b[0:64, :], wuk[:, cc, h, :], cT[:, cc, :], start=(cc == 0), stop=(cc == 1))
                    s_ = stg[si % 4]
                    si += 1
                    k.copy("act" if si % 2 else "dve", s_.v(), b[0:64, :])
                    k.dma(kT[h][:, blk * 512:(blk + 1) * 512], s_.v())
            k.barrier(reset=[qT, kT, iqT, qmT])
            k.release(xt + stg)
        with ExitStack() as es:
            cb = k.sb([128, 128], F32, es=es)
            k.dma(cb.v(), d["c_cbias"].v())
            GRP = 4
            acc = [k.sb([128, S_], F32, es=es) for _ in range(GRP)]
            iq = [k.sb([64, 8, 128], BF16, es=es) for _ in range(2)]
            rl = [k.sb([128, 512], F32, es=es) for _ in range(3)]
            junk = k.sb([128, S_], BF16, es=es)
            mk = [k.sb([128, S_], BF16, es=es) for _ in range(2)]
            mst = [k.sb([128, S_], BF16, es=es) for _ in range(2)]
            smt = [k.sb([128, 8], F32, es=es) for _ in range(GRP)]
            ri = 0
            for g0 in range(0, NT_, GRP):
                tiles = list(range(g0, min(g0 + GRP, NT_)))
                for j in tiles:
                    a = acc[j % GRP]
                    q_ = iq[j % 2]
                    k.dma(q_.v(), iqT[:, :, j * 128:(j + 1) * 128].re("h e t -> e h t"))
                    W = (j + 1) * 128
                    for k0 in range(0, W, 512):
                        w_ = min(512, W - k0)
                        for h in range(8):
                            b = bank(c)
                            k.mm(b[:, 0:w_], q_[:, h, :], ikT[:, k0:k0 + w_])
                            r_ = rl[ri % 3]
                            ri += 1
                            k.act(r_[:, 0:w_], b[:, 0:w_], AF.Relu, scale=absw[:, j, h:h + 1])
                            if h == 0:
                                k.ts("dve", a[:, k0:k0 + w_], r_[:, 0:w_], sgnw[:, j, 0:1], None, op0=ALU.mult)
                            else:
                                k.stt("dve", a[:, k0:k0 + w_], r_[:, 0:w_], sgnw[:, j, h:h + 1], a[:, k0:k0 + w_], ALU.mult, ALU.add)
                bis = [j for j in tiles if (j + 1) * 128 > c.topk]
                for j in tiles:
                    a = acc[j % GRP]
                    W = (j + 1) * 128
                    jl = j % GRP
                    if (j + 1) * 128 > c.topk:
                        k.op("dve", (lambda a=a, W=W, jl=jl: lambda e: e.tensor_reduce(out=smt[jl][:, 5:6].ap, in_=a[:, 0:W].ap, axis=AX.X, op=ALU.max))(), [a], [smt[jl]])
                        k.op("dve", (lambda a=a, W=W, jl=jl: lambda e: e.tensor_reduce(out=smt[jl][:, 6:7].ap, in_=a[:, 0:W].ap, axis=AX.X, op=ALU.min))(), [a], [smt[jl]])
                        k.ts("dve", smt[jl][:, 0:1], smt[jl][:, 6:7], -1e-3, None, op0=ALU.add)
                        k.stt("dve", smt[jl][:, 1:2], smt[jl][:, 5:6], 2e-3, smt[jl][:, 6:7], ALU.add, ALU.subtract)
                    else:
                        k.memset("dve", smt[jl][:, 0:1], -1.0e29)
                    k.tt("pool", a[:, j * 128:W], a[:, j * 128:W], cb.v(), ALU.add)
                NIT = 15
                for it in range(NIT):
                    cf = 2.0 ** -(it + 1)
                    for j in bis:
                        jl = j % GRP
                        k.stt("dve", smt[jl][:, 2:3], smt[jl][:, 1:2], cf, smt[jl][:, 0:1], ALU.mult, ALU.add)
                    for j in bis:
                        jl = j % GRP
                        W = (j + 1) * 128
                        k.ts("dve", junk[:, 0:W], acc[jl][:, 0:W], smt[jl][:, 2:3], None, op0=ALU.is_ge, op1=ALU.add, accum=smt[jl][:, 3:4])
                    for j in bis:
                        jl = j % GRP
                        k.ts("dve", smt[jl][:, 4:5], smt[jl][:, 3:4], c.topk - 0.5, cf, op0=ALU.is_ge, op1=ALU.mult)
                    for j in bis:
                        jl = j % GRP
                        k.stt("dve", smt[jl][:, 0:1], smt[jl][:, 4:5], smt[jl][:, 1:2], smt[jl][:, 0:1], ALU.mult, ALU.add)
                for j in tiles:
                    jl = j % GRP
                    W = (j + 1) * 128
                    m_ = mk[j % 2]
                    ms = mst[j % 2]
                    k.ts("dve", m_[:, 0:W], acc[jl][:, 0:W], smt[jl][:, 0:1], None, op0=ALU.is_ge)
                    if c.dbg == "idx":
                        dbt = k.sb([128, 16], F32, es=es, name=f"dbt{j}")
                        k.copy("dve", dbt[:, 0:8], smt[jl].v())
                        k.op("dve", (lambda o=dbt[:, 8:9], x=m_[:, 0:W]: lambda e: e.tensor_reduce(out=o.ap, in_=x.ap, axis=AX.X, op=ALU.add))(), [m_], [dbt])
                        k.ts("dve", junk[:, 0:W], acc[jl][:, 0:W], smt[jl][:, 0:1], None, op0=ALU.is_ge, op1=ALU.add, accum=dbt[:, 9:10])
                        k.dma(xout[j * 128:(j + 1) * 128, 0:16], dbt.v())
                        k.dma(xout[j * 128:(j + 1) * 128, 16:16 + min(W, 1008)], acc[jl][:, 0:min(W, 1008)])
                    for i0 in range(0, j + 1, 8):
                        n_ = min(8, j + 1 - i0)
                        b = bank(c)
                        pb = b.v().bitcast(BF16)
                        for ii in range(n_):
                            k.tr(pb[:, ii * 128:(ii + 1) * 128], m_[:, (i0 + ii) * 128:(i0 + ii + 1) * 128], c.identb.v())
                        k.copy("act", ms[:, i0 * 128:(i0 + n_) * 128], pb[:, 0:n_ * 128])
                    k.dma(MT[j][:, 0:W], ms[:, 0:W])
            k.barrier(reset=[MT])
            k.release(iq + mst)
        if c.dbg == "idx":
            with ExitStack() as es:
                mb_ = k.sb([128, 1024], BF16, es=es)
                mf_ = k.sb([128, 1024], F32, es=es)
                k.dma(mb_.v(), MT[7][:, 0:1024])
                k.copy("dve", mf_.v(), mb_.v())
                k.dma(xout[0:128, :], mf_.v())
                k.barrier()
            return
        with ExitStack() as es:
            wo = load_w_bf16(k, c, es, d["w_o"][0], D, D, "wo0")
            rb = k.sb([32, 12], F32, es=es)
            k.dma(rb.v(), d["rel_bias"].v())
            ohr = k.sb([32, 512], F32, es=es)
            k.dma(ohr.v(), d["c_ohr"].v())
            oh31 = k.sb([32, 128], F32, es=es)
            k.dma(oh31.v(), d["c_oh31"].v())
            b31 = k.sb([128, 12], F32, es=es)
            b = bank(c)
            k.mm(b[:, 0:12], oh31.v(), rb.v())
            k.copy("dve", b31.v(), b[:, 0:12])
            Tn = [k.sb([128, 12, 128], BF16, es=es) for _ in range(2)]
            for kk in range(2):
                base = 0 if kk == 0 else 4
                pv = psv(c, base, 4)
                for t in range(128):
                    o0 = 255 - 128 * kk - t
                    k.mm(V([c.banks[base + t // 32]], c.psall[:, base * 512 + t * 16: base * 512 + t * 16 + 12]), ohr[:, o0:o0 + 128], rb.v())
                k.act(Tn[kk].v().re("p h t -> p t h"), pv.re("p (t e) -> p t e", e=16)[:, :, 0:12], AF.Exp)
            qh = [k.sb([64, 512], BF16, es=es) for _ in range(2)]
            kh = [k.sb([64, S_], BF16, es=es) for _ in range(2)]
            qm = k.sb([64, 4, 512], BF16, es=es)
            mkJ = [k.sb([128, NT_, 512], BF16, es=es) for _ in range(1)]
            pt = [k.sb([128, 512], BF16, es=es) for _ in range(3)]
            cat = k.sb([128, 4, D], BF16, es=es)
            rden = k.sb([128, 4], F32, es=es)
            wk = {"pt": pt, "rden": rden, "catT": k.sb([128, D], BF16, es=es),
                  "xres": [k.sb([128, D], F32, es=es) for _ in range(2)], "xi": 0}
            pi = 0
            hi = 0
            sbi = 0
            for J in range(NB):
                nst = 4 * J + 4
                mJ = mkJ[0]
                for jl in range(4):
                    jj = 4 * J + jl
                    k.dma(mJ[:, 0:jj + 1, jl * 128:(jl + 1) * 128], MT[jj][:, 0:(jj + 1) * 128].re("p (i t) -> p i t", t=128))
                for h in range(12):
                    q_ = qh[hi % 2]
                    k_ = kh[hi % 2]
                    hi += 1
                    k.dma(q_.v(), qT[h][:, J * 512:(J + 1) * 512])
                    k.dma(k_[:, 0:nst * 128], kT[h][:, 0:nst * 128])
                    ob = c.banks[hi % 2]
                    for i in range(nst):
                        dl = max(0, i - 4 * J)
                        c0 = dl * 128
                        sb_ = c.banks[2 + sbi % 6]
                        sbi += 1
                        k.mm(sb_[:, c0:512], k_[:, i * 128:(i + 1) * 128], q_[:, c0:512])
                        p = pt[pi % 3]
                        pi += 1
                        k.act(p[:, c0:512], sb_[:, c0:512], AF.Exp, bias=b31[:, h:h + 1], scale=0.125)
                        k.tt("dve", p[:, c0:512], p[:, c0:512], mJ[:, i, c0:512], ALU.mult)
                        for jl in range(dl, 4):
                            jj = 4 * J + jl
                            if jj - i <= 1:
                                k.tt("pool", p[:, jl * 128:(jl + 1) * 128], p[:, jl * 128:(jl + 1) * 128], Tn[jj - i][:, h, :], ALU.mult)
                        for jl in range(dl, 4):
                            jj = 4 * J + jl
                            k.mm(ob[:, jl * 65:(jl + 1) * 65], p[:, jl * 128:(jl + 1) * 128], Vres[:, i, h, :], start=(i == 0 and jl == 0), stop=(i == jj))
                    ov = ob[:, 0:260].re("p (j e) -> p j e", e=65)
                    if c.dbg == "cat" and J == 1 and h == 0:
                        dbo = k.sb([128, 1024], F32, es=es, name="dbo")
                        k.memset("pool", dbo.v(), 0.0)
                        k.copy("dve", dbo[:, 0:260], ob[:, 0:260])
                        k.copy("dve", dbo[:, 512:1024], p.v())
                        k.dma(xout[0:128, :], dbo.v())
                        dbo2 = k.sb([128, 1024], F32, es=es, name="dbo2")
                        k.copy("dve", dbo2[:, 0:512], mJ[:, 7, :])
                        k.copy("dve", dbo2[:, 512:1024], mJ[:, 6, :])
                        k.dma(xout[128:256, :], dbo2.v())
                    k.recip(rden.v(), ov[:, :, 64])
                    k.tt("dve", cat[:, :, h * 64:(h + 1) * 64], ov[:, :, 0:64], rden.v().us(2).bc([128, 4, 64]), ALU.mult)
                k.dma(qm.v(), qmT[:, :, J * 512:(J + 1) * 512].re("h e t -> e h t"))
                mem_attention(k, c, 0, qm.v(), 4, cat.v(), wk)
                for jl in range(4):
                    if getattr(c, "dbg", None) == "cat":
                        xt_ = wk["xres"][jl % 2]
                        k.copy("dve", xt_.v(), cat[:, jl, :])
                        k.dma(xout[(4 * J + jl) * 128:(4 * J + jl + 1) * 128, :], xt_.v())
                        continue
                    out_proj_residual(k, c, cat[:, jl, :], wo, xin, xout, (4 * J + jl) * 128, wk)
            k.barrier(reset=[xout])
            k.release(qh + kh + [qm] + mkJ + wk["xres"])
    k.barrier()


def peer_layer(k, c, li, xin, xout, final):
    d = c.d
    S_ = c.S
    TB = 256
    NBK = S_ // TB
    NCH = 128
    uT = k.dram(f"uT{li}", [NCH, 128, 8, 128], BF16)
    vb = k.dram(f"vb{li}", [16384, D], BF16)
    lb = [4]

    def lbank():
        b = c.banks[4 + lb[0] % 4]
        lb[0] += 1
        return b

    with ExitStack() as es:
        m0 = k.mark()
        uf = [k.sb([128, 2, D], F32, es=es) for _ in range(2)]
        vf = [k.sb([128, 2, D], F32, es=es) for _ in range(2)]
        ubf = [k.sb([128, 2, D], BF16, es=es) for _ in range(2)]
        vbf = [k.sb([128, 2, D], BF16, es=es) for _ in range(2)]
        uo = [k.sb([128, 2, 8, 128], BF16, es=es) for _ in range(2)]
        for a2 in range(NCH // 2):
            u_, v_, ub_, vb_, uo_ = uf[a2 % 2], vf[a2 % 2], ubf[a2 % 2], vbf[a2 % 2], uo[a2 % 2]
            k.dma(u_.v(), d["peer_u"][li, a2 * 256:(a2 + 1) * 256, :].re("(a p) f -> p a f", a=2))
            k.dma(v_.v(), d["peer_v"][li, a2 * 256:(a2 + 1) * 256, :].re("(a p) f -> p a f", a=2))
            k.copy("dve", ub_.v(), u_.v())
            k.copy("act", vb_.v(), v_.v())
            for ai in range(2):
                b = bank(c)
                pb = b.v().bitcast(BF16)
                for kk in range(8):
                    k.tr(pb[:, kk * 128:(kk + 1) * 128], ub_[:, ai, kk * 128:(kk + 1) * 128], c.identb.v())
                k.copy("act" if ai else "dve", uo_[:, ai, :, :], pb.re("p (k e) -> p k e", k=8))
            k.dma(uT[a2 * 2:a2 * 2 + 2].re("a p k e -> p a k e"), uo_.v())
            k.dma(vb[a2 * 256:(a2 + 1) * 256, :].re("(a p) f -> p a f", a=2), vb_.v())
        k.barrier()
        k.release_since(m0)
    with ExitStack() as es:
        m0 = k.mark()
        wq = load_w_bf16(k, c, es, d["peer_w_q"][li], D, 2048, f"wq{li}")
        KT = k.sb([128, 2, 128], BF16, es=es)
        skf = k.sb([128, 128], F32, es=es)
        for i in range(2):
            k.dma(skf.v(), d["peer_sub_keys"][li, i])
            b = lbank()
            k.tr(b[:, 0:128], skf.v(), c.identf.v())
            k.copy("dve", KT[:, i, :], b[:, 0:128])
        gfull = k.sb([128, D], F32, es=es)
        k.dma(gfull.v(), d["ffn_norm"][li, :].pbc(128))
        if final:
            gfin = k.sb([128, D], F32, es=es)
            k.dma(gfin.v(), d["final_norm"].v().pbc(128))
        iota = k.sb([128, 128], F32, es=es)
        k.dma(iota.v(), d["c_iota"].v())
        Gbuf = k.sb([128, 128, TB], BF16, es=es)
        hnT = k.sb([128, 8, TB], BF16, es=es)
        qTall = k.sb([128, 16, TB], BF16, es=es)
        xkeep = [k.sb([128, D], F32, es=es) for _ in range(2)]
        sc = k.sb([128, 2048], F32, es=es)
        cand = k.sb([128, 8, 256], F32, es=es)
        tmpg = k.sb([128, 16, 128], F32, es=es)
        hb = k.sb([128, D], BF16, es=es)
        ss = k.sb([128, 1], F32, es=es)
        rs = k.sb([128, 1], F32, es=es)
        ts_ = k.sb([128, 8, 2, 16], F32, es=es)
        ix = k.sb([128, 8, 16], U32, es=es)
        best = k.sb([128, 8, 16], F32, es=es)
        eb = k.sb([128, 8, 16], F32, es=es)
        Z = k.sb([128, 8], F32, es=es)
        tau = k.sb([128, 8], F32, es=es)
        tok3 = [k.sb([128, 8, 16], F32, es=es) for _ in range(3)]
        jT = [k.sb([128, TB], F32, es=es) for _ in range(3)]
        ub = [k.sb([128, 2, 8, 128], BF16, es=es) for _ in range(3)]
        vbuf = [k.sb([128, 2, D], BF16, es=es) for _ in range(3)]
        oa2 = [k.sb([128, 16, 128], BF16, es=es) for _ in range(2)]
        q1r_single = k.sb([128, 16, 128], BF16, es=es)
        q1r2 = [q1r_single, q1r_single]
        e1 = [k.sb([128, 512], BF16, es=es) for _ in range(2)]
        mm_ = [k.sb([128, 512], BF16, es=es) for _ in range(2)]
        ag = [k.sb([128, TB], BF16, es=es) for _ in range(2)]
        for tb in range(NBK):
            for tl in range(2):
                t0 = tb * TB + tl * 128
                xk = xkeep[tl]
                k.dma(xk.v(), xin[t0:t0 + 128, :])
                rmsnorm_tile(k, c, xk.v(), gfull.v(), hb.v(), D, sc[:, 0:D], ss.v(), rs.v())
                b = lbank()
                pb = b.v().bitcast(BF16)
                for kk in range(8):
                    k.tr(pb[:, kk * 128:(kk + 1) * 128], hb[:, kk * 128:(kk + 1) * 128], c.identb.v())
                k.copy("act", hnT[:, :, tl * 128:(tl + 1) * 128], pb.re("p (k t) -> p k t", k=8))
            for g2 in range(8):
                b = lbank()
                for gg in range(2):
                    g = g2 * 2 + gg
                    for kk in range(8):
                        k.mm(b[:, gg * TB:(gg + 1) * TB], wq[:, kk, g * 128:(g + 1) * 128], hnT[:, kk, :], start=(kk == 0), stop=(kk == 7))
                k.copy("act" if g2 % 2 else "dve", qTall[:, g2 * 2:g2 * 2 + 2, :], b.v().re("p (g t) -> p g t", g=2))
            for tl in range(2):
                sbk = [lbank() for _ in range(4)]
                for g in range(16):
                    k.mm(sbk[g // 4][:, (g % 4) * 128:(g % 4 + 1) * 128], qTall[:, g, tl * 128:(tl + 1) * 128], KT[:, g % 2, :])
                for q4 in range(4):
                    k.copy("act" if q4 % 2 else "dve", sc[:, q4 * 512:(q4 + 1) * 512], sbk[q4].v())
                G16 = [(g, g // 2, g % 2, sc[:, g * 128:(g + 1) * 128]) for g in range(16)]
                for (g, h, i, sg) in G16:
                    k.op("dve", (lambda o=ts_[:, h, i, 0:8], x=sg: lambda e: e.max(out=o.ap, in_=x.ap))(), [sc], [ts_])
                for (g, h, i, sg) in G16:
                    k.op("dve", (lambda o=tmpg[:, g, :], m=ts_[:, h, i, 0:8], x=sg: lambda e: e.match_replace(out=o.ap, in_to_replace=m.ap, in_values=x.ap, imm_value=NEG))(), [sc, ts_], [tmpg])
                for (g, h, i, sg) in G16:
                    if i == 0:
                        k.op("dve", (lambda o=ix[:, h, 0:8], m=ts_[:, h, i, 0:8], x=sg: lambda e: e.max_index(out=o.ap, in_max=m.ap, in_values=x.ap))(), [sc, ts_], [ix])
                for (g, h, i, sg) in G16:
                    k.op("dve", (lambda o=ts_[:, h, i, 8:16], x=tmpg[:, g, :]: lambda e: e.max(out=o.ap, in_=x.ap))(), [tmpg], [ts_])
                for (g, h, i, sg) in G16:
                    if i == 0:
                        k.op("dve", (lambda o=ix[:, h, 8:16], m=ts_[:, h, i, 8:16], x=tmpg[:, g, :]: lambda e: e.max_index(out=o.ap, in_max=m.ap, in_values=x.ap))(), [tmpg, ts_], [ix])
                k.tt("pool", cand.v().re("p h (a b) -> p h a b", a=16), ts_[:, :, 0, :].us(3).bc([128, 8, 16, 16]), ts_[:, :, 1, :].us(2).bc([128, 8, 16, 16]), ALU.add)
                for h in range(8):
                    k.op("dve", (lambda o=best[:, h, 0:8], x=cand[:, h, :]: lambda e: e.max(out=o.ap, in_=x.ap))(), [cand], [best])
                for h in range(8):
                    k.op("dve", (lambda o=tmpg[:, 2 * h:2 * h + 2, :].re("p a b -> p (a b)"), m=best[:, h, 0:8], x=cand[:, h, :]: lambda e: e.match_replace(out=o.ap, in_to_replace=m.ap, in_values=x.ap, imm_value=NEG))(), [cand, best], [tmpg])
                for h in range(8):
                    k.op("dve", (lambda o=best[:, h, 8:16], x=tmpg[:, 2 * h:2 * h + 2, :].re("p a b -> p (a b)"): lambda e: e.max(out=o.ap, in_=x.ap))(), [tmpg], [best])
                thr, coef, idxf = tok3
                k.ts("dve", tau.v(), best[:, :, 15], -1e-5, None, op0=ALU.add)
                k.tt("dve", eb.v(), best.v(), best[:, :, 0:1].bc([128, 8, 16]), ALU.subtract)
                k.act(eb.v(), eb.v(), AF.Exp)
                k.op("dve", lambda e: e.tensor_reduce(out=Z.v().ap, in_=eb.v().ap, axis=AX.X, op=ALU.add), [eb], [Z])
                k.recip(Z.v(), Z.v())
                k.tt("dve", coef.v(), ts_[:, :, 0, :], best[:, :, 0:1].bc([128, 8, 16]), ALU.subtract)
                k.act(coef.v(), coef.v(), AF.Exp)
                k.tt("dve", coef.v(), coef.v(), Z.v().us(2).bc([128, 8, 16]), ALU.mult)
                k.tt("dve", thr.v(), tau.v().us(2).bc([128, 8, 16]), ts_[:, :, 0, :], ALU.subtract)
                k.copy("dve", idxf.v(), ix.v())
                for q3 in range(3):
                    b = lbank()
                    k.tr(b[:, 0:128], tok3[q3].v().re("p h a -> p (h a)"), c.identf.v())
                    k.copy("act", jT[q3][:, tl * 128:(tl + 1) * 128], b[:, 0:128])
            thrT, coefT, idxT = jT
            if c.dbg == "peer_p1":
                break
            for t16 in range(TB // 16):
                t0 = t16 * 16
                oa = oa2[t16 % 2]
                q1r = q1r2[t16 % 2]
                k.copy("dve", q1r.v().re("p t (h r) -> p t h r", r=16),
                       qTall.v().re("p (h i) t -> p i t h", i=2)[:, 1, t0:t0 + 16, :].us(3).bc([128, 16, 8, 16]))
                k.tt("dve", oa.v(), iota.v().us(1).bc([128, 16, 128]), idxT[:, t0:t0 + 16].us(2).bc([128, 16, 128]), ALU.is_equal)
                k.tt("dve", oa.v(), oa.v(), coefT[:, t0:t0 + 16].us(2).bc([128, 16, 128]), ALU.mult)
                def emit_s1(t4):
                    sbk = lbank()
                    for q in range(4):
                        k.mm(sbk[:, q * 128:(q + 1) * 128], q1r[:, t4 * 4 + q, :], KT[:, 1, :])
                    return sbk
                pend = emit_s1(0)
                k.act(e1[0].v(), pend.v(), AF.Exp)
                for t4 in range(4):
                    tt0 = t0 + t4 * 4
                    sbk = pend
                    if t4 + 1 < 4:
                        pend = emit_s1(t4 + 1)
                        k.act(e1[(t4 + 1) % 2].v(), pend.v(), AF.Exp)
                    e_ = e1[t4 % 2]
                    m_ = mm_[t4 % 2]
                    k.tt("dve", m_.v().re("p (t b) -> p t b", t=4), sbk.v().re("p (t b) -> p t b", t=4), thrT[:, tt0:tt0 + 4].us(2).bc([128, 4, 128]), ALU.is_ge)
                    k.tt("pool", m_.v(), m_.v(), e_.v(), ALU.mult)
                    gbk = lbank()
                    for q in range(4):
                        k.mm(gbk[:, q * 128:(q + 1) * 128], m_[:, q * 128:(q + 1) * 128], oa[:, t4 * 4 + q, :])
                    k.copy("act", Gbuf[:, :, tt0:tt0 + 4].re("p a t -> p t a"), gbk.v().re("p (t a) -> p t a", t=4))
            if c.dbg == "peer_p2":
                break
            def load_pair(a2):
                k.dma(ub[a2 % 3].v(), uT[a2 * 2:a2 * 2 + 2].re("a p k e -> p a k e"))
                k.dma(vbuf[a2 % 3].v(), vb[a2 * 256:(a2 + 1) * 256, :].re("(a p) f -> p a f", a=2))

            def emit_A(a):
                a2, ai = a // 2, a % 2
                if ai == 0 and a2 + 1 < NCH // 2:
                    load_pair(a2 + 1)
                ab = lbank()
                for kk in range(8):
                    k.mm(ab[:, 0:TB], ub[a2 % 3][:, ai, kk, :], hnT[:, kk, :], start=(kk == 0), stop=(kk == 7))
                return ab
            load_pair(0)
            pend = emit_A(0)
            for a in range(NCH):
                a2, ai = a // 2, a % 2
                ab = pend
                if a + 1 < NCH:
                    pend = emit_A(a + 1)
                g_ = ag[a % 2]
                k.act(g_.v(), ab[:, 0:TB], AF.Gelu)
                k.tt("dve", g_.v(), g_.v(), Gbuf[:, a, :], ALU.mult)
                for tl in range(2):
                    for half in range(2):
                        k.mm(c.banks[tl * 2 + half].v(), g_[:, tl * 128:(tl + 1) * 128], vbuf[a2 % 3][:, ai, half * 512:(half + 1) * 512], start=(a == 0), stop=(a == NCH - 1))
            for tl in range(2):
                t0 = tb * TB + tl * 128
                xk = xkeep[tl]
                for half in range(2):
                    k.tt("dve", xk[:, half * 512:(half + 1) * 512], xk[:, half * 512:(half + 1) * 512], c.banks[tl * 2 + half].v(), ALU.add)
                if final:
                    k.act(sc[:, 0:D], xk.v(), AF.Square, accum=ss.v())
                    k.act(rs.v(), ss.v(), AF.Sqrt, bias=c.eps.v(), scale=1.0 / D)
                    k.recip(rs.v(), rs.v())
                    k.stt("dve", xk.v(), xk.v(), rs.v(), gfin.v(), ALU.mult, ALU.mult)
                k.dma(xout[t0:t0 + 128, :], xk.v())
            k.barrier()
        k.barrier()
        k.release_since(m0)


def layer1_mixer(k, c, xin, xout):
    d = c.d
    S_ = c.S
    NT_ = S_ // 128
    with ExitStack() as es:
        m0 = k.mark()
        win = load_w_bf16(k, c, es, d["b_w_in"], D, B_IN, "b_win")
        wo = load_w_bf16(k, c, es, d["w_o"][1], D, D, "wo1")
        gfull = k.sb([128, D], F32, es=es)
        k.dma(gfull.v(), d["mix_norm"][1, :].pbc(128))
        onorm = k.sb([128, 768], F32, es=es)
        k.dma(onorm.v(), d["b_out_norm"][0, :].pbc(128))
        cw5 = k.sb([5, 1280], F32, es=es)
        k.dma(cw5[0:4, :], d["b_conv_w"].v())
        k.dma(cw5[4:5, :], d["b_conv_b"].v())
        cwb = k.sb([128, 10, 5], F32, es=es)
        b = bank(c)
        for ck in range(10):
            k.tr(b[:, ck * 8:ck * 8 + 5], cw5[:, ck * 128:(ck + 1) * 128], c.identf[0:5, 0:5])
        k.copy("dve", cwb.v(), b[:, 0:80].re("p (k e) -> p k e", e=8)[:, :, 0:5])
        dtb = k.sb([128, 12], F32, es=es)
        k.dma(dtb.v(), d["b_dt_bias"][0, :].pbc(128))
        arow = k.sb([128, 12], F32, es=es)
        k.dma(arow.v(), d["b_a_log"][0, :].pbc(128))
        k.act(arow.v(), arow.v(), AF.Exp)
        k.ts("dve", arow.v(), arow.v(), -1.0, None, op0=ALU.mult)
        dsk = k.sb([128, 12], F32, es=es)
        k.dma(dsk.v(), d["b_d_skip"][0, :].pbc(128))
        Dfull = k.sb([128, 12, 64], F32, es=es)
        k.copy("dve", Dfull.v(), dsk.v().us(2).bc([128, 12, 64]))
        triu = k.sb([128, 128], F32, es=es)
        k.dma(triu.v(), d["c_triu"].v())
        negtri = k.sb([128, 128], F32, es=es)
        k.ts("dve", negtri.v(), triu.v(), -1.0, -NEG, op0=ALU.add, op1=ALU.mult)
        ones = k.sb([128, 128], F32, es=es)
        k.memset("pool", ones.v(), 1.0)
        hst = k.sb([128, 12, 64], F32, es=es)
        hbf = k.sb([128, 12, 64], BF16, es=es)
        k.memset("pool", hst.v(), 0.0)
        k.memset("pool", hbf.v(), 0.0)
        xr = k.sb([128, 10, 131], F32, es=es)
        k.memset("pool", xr.v(), 0.0)
        xt = [k.sb([128, D], F32, es=es) for _ in range(2)]
        scr = k.sb([128, D], F32, es=es)
        hb = k.sb([128, D], BF16, es=es)
        ss = k.sb([128, 1], F32, es=es)
        rs = k.sb([128, 1], F32, es=es)
        hT = k.sb([128, 8, 128], BF16, es=es)
        acc = k.sb([128, 10, 128], F32, es=es)
        xc = k.sb([128, 10, 128], BF16, es=es)
        xtok = k.sb([128, D], BF16, es=es)
        zs = k.sb([128, 768], F32, es=es)
        dt = k.sb([128, 12], F32, es=es)
        da = k.sb([128, 12], F32, es=es)
        acs = k.sb([128, 12], F32, es=es)
        dte = k.sb([128, 12], F32, es=es)
        qm = k.sb([64, 4, 128], BF16, es=es)
        Uda = k.sb([128, 12, 128], F32, es=es)
        tmp = k.sb([128, 12, 128], F32, es=es)
        Lexp = k.sb([128, 12, 128], BF16, es=es)
        Eall = k.sb([128, 12, 128], F32, es=es)
        cbt = k.sb([128, 2, 128], BF16, es=es)
        MTm = k.sb([128, 12, 128], BF16, es=es)
        CTs = k.sb([128, 12, 128], BF16, es=es)
        Xd = k.sb([128, 12, 64], BF16, es=es)
        Xdd = k.sb([128, 12, 64], BF16, es=es)
        yv = k.sb([128, 768], F32, es=es)
        cat = k.sb([128, 1, D], BF16, es=es)
        wk = {"pt": [k.sb([128, 512], BF16, es=es) for _ in range(2)], "rden": k.sb([128, 4], F32, es=es),
              "catT": k.sb([128, D], BF16, es=es), "xres": [k.sb([128, D], F32, es=es) for _ in range(2)], "xi": 0}
        for j in range(NT_):
            x_ = xt[j % 2]
            k.dma(x_.v(), xin[j * 128:(j + 1) * 128, :])
            rmsnorm_tile(k, c, x_.v(), gfull.v(), hb.v(), D, scr.v(), ss.v(), rs.v())
            b = bank(c)
            pb = b.v().bitcast(BF16)
            for kk in range(8):
                k.tr(pb[:, kk * 128:(kk + 1) * 128], hb[:, kk * 128:(kk + 1) * 128], c.identb.v())
            k.copy("act", hT.v(), pb.re("p (k t) -> p k t", k=8))
            for c0 in range(0, 10, 4):
                n_ = min(4, 10 - c0)
                b = bank(c)
                for ci in range(n_):
                    ck = c0 + ci
                    for kk in range(8):
                        k.mm(b[:, ci * 128:(ci + 1) * 128], win[:, kk, 768 + ck * 128:768 + (ck + 1) * 128], hT[:, kk, :], start=(kk == 0), stop=(kk == 7))
                k.copy("act", xr[:, c0:c0 + n_, 3:131], b[:, 0:n_ * 128].re("p (k t) -> p k t", k=n_))
            for ck in range(10):
                k.ts("dve", acc[:, ck, :], xr[:, ck, 0:128], cwb[:, ck, 0:1], cwb[:, ck, 4:5], op0=ALU.mult, op1=ALU.add)
                for jj in range(1, 4):
                    k.stt("dve", acc[:, ck, :], xr[:, ck, jj:jj + 128], cwb[:, ck, jj:jj + 1], acc[:, ck, :], ALU.mult, ALU.add)
            k.copy("pool", xr[:, :, 0:3], xr[:, :, 128:131])
            k.act(xc.v(), acc.v(), AF.Silu)
            b = bank(c)
            pb = b.v().bitcast(BF16)
            for ck in range(8):
                k.tr(pb[:, ck * 128:(ck + 1) * 128], xc[:, ck, :], c.identb.v())
            k.copy("act", xtok.v(), pb)
            b0 = bank(c)
            b1 = bank(c)
            for kk in range(8):
                k.mm(b0.v(), hT[:, kk, :], win[:, kk, 0:512], start=(kk == 0), stop=(kk == 7))
            for kk in range(8):
                k.mm(b1[:, 0:256], hT[:, kk, :], win[:, kk, 512:768], start=(kk == 0), stop=(kk == 7))
            k.act(zs[:, 0:512], b0.v(), AF.Silu)
            k.act(zs[:, 512:768], b1[:, 0:256], AF.Silu)
            b = bank(c)
            for kk in range(8):
                k.mm(b[:, 0:12], hT[:, kk, :], win[:, kk, 2048:2060], start=(kk == 0), stop=(kk == 7))
            k.tt("dve", dt.v(), b[:, 0:12], dtb.v(), ALU.add)
            k.act(dt.v(), dt.v(), AF.Exp)
            k.act(dt.v(), dt.v(), AF.Ln, bias=c.one.v())
            k.tt("dve", da.v(), dt.v(), arow.v(), ALU.mult)
            b = bank(c)
            for h in range(4):
                for kk in range(8):
                    k.mm(b[0:64, h * 128:(h + 1) * 128], win[:, kk, 2060 + h * 64:2060 + (h + 1) * 64], hT[:, kk, :], start=(kk == 0), stop=(kk == 7))
            k.copy("act", qm.v(), b[0:64, :].re("p (h t) -> p h t", h=4))
            b = bank(c)
            k.mm(b[:, 0:12], triu.v(), da.v())
            k.copy("dve", acs.v(), b[:, 0:12])
            k.tt("dve", Uda.v(), triu.v().us(1).bc([128, 12, 128]), da.v().us(2).bc([128, 12, 128]), ALU.mult)
            arow_ps = psv(c, 0, 3)
            for q in range(3):
                k.mm(c.banks[q].v(), ones.v(), Uda[:, q * 4:(q + 1) * 4, :].re("p h l -> p (h l)"))
            ar3 = arow_ps.re("p (h l) -> p h l", h=12)
            k.tt("dve", tmp.v(), ar3, acs.v().us(2).bc([128, 12, 128]), ALU.subtract)
            k.tt("pool", tmp.v(), tmp.v(), negtri.v().us(1).bc([128, 12, 128]), ALU.add)
            k.act(Lexp.v(), tmp.v(), AF.Exp)
            k.act(Eall.v(), ar3, AF.Exp)
            k.tt("dve", dte.v(), ar3[:, :, 127], acs.v(), ALU.subtract)
            k.act(dte.v(), dte.v(), AF.Exp)
            b = bank(c)
            for g in range(2):
                k.mm(b[:, g * 128:(g + 1) * 128], xc[:, 6 + g, :], xc[:, 8 + g, :])
            k.copy("act", cbt.v(), b[:, 0:256].re("p (g l) -> p g l", g=2))
            k.tt("dve", MTm.v().re("p (g r) l -> p g r l", g=2), Lexp.v().re("p (g r) l -> p g r l", g=2), cbt.v().us(2).bc([128, 2, 6, 128]), ALU.mult)
            k.tt("pool", Xd.v(), xtok[:, 0:768].re("p (r e) -> p r e", e=64), dt.v().us(2).bc([128, 12, 64]), ALU.mult)
            k.tt("dve", CTs.v().re("p (g r) l -> p g r l", g=2), Eall.v().re("p (g r) l -> p g r l", g=2), xc[:, 8:10, :].us(2).bc([128, 2, 6, 128]), ALU.mult)
            k.tt("pool", Xdd.v(), Xd.v(), dte.v().us(2).bc([128, 12, 64]), ALU.mult)
            ybA = bank(c)
            ybB = bank(c)
            for r in range(12):
                yb = ybA if r < 8 else ybB
                cs_ = (r % 8) * 64
                k.mm(yb[:, cs_:cs_ + 64], MTm[:, r, :], Xd[:, r, :], start=(r % 8 == 0), stop=False)
                k.mm(yb[:, cs_:cs_ + 64], CTs[:, r, :], hbf[:, r, :], start=False, stop=True)
            sbA = bank(c)
            sbB = bank(c)
            for r in range(12):
                sbk = sbA if r < 8 else sbB
                cs_ = (r % 8) * 64
                g = r // 6
                k.mm(sbk[:, cs_:cs_ + 64], xtok[:, 768 + g * 128:768 + (g + 1) * 128], Xdd[:, r, :], start=(r % 8 == 0), stop=True)
            k.tt("pool", yv.v(), xtok[:, 0:768], Dfull.v().re("p r e -> p (r e)"), ALU.mult)
            k.tt("dve", yv[:, 0:512], yv[:, 0:512], ybA.v(), ALU.add)
            k.tt("dve", yv[:, 512:768], yv[:, 512:768], ybB[:, 0:256], ALU.add)
            k.tt("dve", yv.v(), yv.v(), zs.v(), ALU.mult)
            k.act(scr[:, 0:768], yv.v(), AF.Square, accum=ss.v())
            k.act(rs.v(), ss.v(), AF.Sqrt, bias=c.eps.v(), scale=1.0 / 768)
            k.recip(rs.v(), rs.v())
            k.stt("dve", cat[:, 0, 0:768], yv.v(), rs.v(), onorm.v(), ALU.mult, ALU.mult)
            k.tt("pool", hst.v(), hst.v(), Eall[:, :, 127:128].bc([128, 12, 64]), ALU.mult)
            k.tt("dve", hst[:, 0:8, :], hst[:, 0:8, :], sbA.v().re("p (r e) -> p r e", e=64), ALU.add)
            k.tt("dve", hst[:, 8:12, :], hst[:, 8:12, :], sbB[:, 0:256].re("p (r e) -> p r e", e=64), ALU.add)
            k.copy("act", hbf.v(), hst.v())
            mem_attention(k, c, 1, qm.v(), 1, cat.v(), wk)
            out_proj_residual(k, c, cat[:, 0, :], wo, xin, xout, j * 128, wk)
        k.barrier()
        k.release_since(m0)

def build_program(S_, stages, dbg=None):
    nc = bass.Bass("TRN2", target_bir_lowering=False)
    c = Ctx()
    c.dbg = dbg
    c.S = S_
    c.topk = min(256, S_ // 4)
    es = ExitStack()
    with es:
        k = K(nc, es)
        c.d = {}

        def inp(name, shape, dt=F32):
            c.d[name] = k.reg(Buf(nc.dram_tensor(name, list(shape), dt, kind="ExternalInput"), name, "dr"))

        inp("x", [S_, D])
        inp("mem", [256, D])
        inp("mem_norm", [D])
        inp("rel_bias", [32, 12])
        inp("mix_norm", [2, D])
        inp("ffn_norm", [2, D])
        inp("final_norm", [D])
        inp("w_o", [2, D, D])
        inp("w_mem_kv", [2, D, 512])
        inp("a_w_in", [D, A_IN])
        inp("a_kv_norm", [1, 256])
        inp("a_w_uk", [12, 256, 64])
        inp("a_w_uv", [12, 256, 64])
        inp("b_w_in", [D, B_IN])
        inp("b_conv_w", [4, 1280])
        inp("b_conv_b", [1, 1280])
        inp("b_dt_bias", [1, 12])
        inp("b_a_log", [1, 12])
        inp("b_d_skip", [1, 12])
        inp("b_out_norm", [1, 768])
        inp("peer_w_q", [2, D, 2048])
        inp("peer_sub_keys", [2, 2, 128, 128])
        inp("peer_u", [2, 16384, D])
        inp("peer_v", [2, 16384, D])
        inp("c_ident", [128, 128])
        inp("c_cbias", [128, 128])
        inp("c_ohr", [32, 512])
        inp("c_oh31", [32, 128])
        inp("c_iota", [128, 128])
        inp("c_triu", [128, 128])
        out = k.reg(Buf(nc.dram_tensor("out", [S_, D], F32, kind="ExternalOutput"), "out", "dr"))
        setup_common(k, c)
        phase_mem(k, c)
        x1 = out if stages == 1 else k.dram("x1", [S_, D], F32)
        if stages == 2:
            peer_layer(k, c, 0, c.d["x"], out, False)
        elif stages == 5:
            layer1_mixer(k, c, c.d["x"], out)
        else:
            layer0_mixer(k, c, c.d["x"], x1)
        if stages == 3:
            layer1_mixer(k, c, x1, out)
        elif stages == 99:
            x2 = k.dram("x2", [S_, D], F32)
            peer_layer(k, c, 0, x1, x2, False)
            x3 = k.dram("x3", [S_, D], F32)
            layer1_mixer(k, c, x2, x3)
            peer_layer(k, c, 1, x3, out, True)
        k.barrier()
        global LASTK
        LASTK = k
        with nc.Block() as block:
            k.emit(block)
    return nc


def t5_bucket_np(dist):
    n = np.maximum(dist, 0)
    me = 16
    nf = np.maximum(n, me).astype(np.float32)
    large = me + (np.log(nf / me) / math.log(128 / me) * (32 - me)).astype(np.int32)
    large = np.minimum(large, 31)
    return np.where(n < me, n, large)


def host_consts():
    cs = {}
    cs["c_ident"] = np.eye(128, dtype=np.float32)
    tt = np.arange(128)
    cs["c_cbias"] = np.where(tt[None, :] <= tt[:, None], 0.0, NEG).astype(np.float32)
    ohr = np.zeros((32, 512), np.float32)
    for m in range(256):
        ohr[t5_bucket_np(np.array(255 - m)), m] += 1.0
        ohr[31, m] -= 1.0
    cs["c_ohr"] = ohr
    oh31 = np.zeros((32, 128), np.float32)
    oh31[31, :] = 1.0
    cs["c_oh31"] = oh31
    cs["c_iota"] = np.tile(np.arange(128, dtype=np.float32)[None, :], (128, 1))
    cs["c_triu"] = (tt[:, None] <= tt[None, :]).astype(np.float32)
    return cs


def make_in_maps(inputs, n_cores, S_):
    cs = host_consts()
    maps = []
    for b in range(n_cores):
        m = {}
        m["x"] = np.ascontiguousarray(inputs["x"][b, :S_])
        m["mem"] = np.ascontiguousarray(inputs["mem"][b])
        for nm in ("mem_norm", "rel_bias", "mix_norm", "ffn_norm", "final_norm", "w_o", "w_mem_kv",
                   "a_kv_norm", "b_conv_b", "b_dt_bias", "b_a_log", "b_d_skip", "b_out_norm",
                   "peer_w_q", "peer_sub_keys", "peer_u", "peer_v"):
            m[nm] = np.ascontiguousarray(inputs[nm])
        m["a_w_in"] = np.ascontiguousarray(inputs["a_w_in"][0])
        m["a_w_uk"] = np.ascontiguousarray(inputs["a_w_uk"][0])
        m["a_w_uv"] = np.ascontiguousarray(inputs["a_w_uv"][0])
        m["b_w_in"] = np.ascontiguousarray(inputs["b_w_in"][0])
        m["b_conv_w"] = np.ascontiguousarray(inputs["b_conv_w"][0])
        m.update(cs)
        maps.append(m)
    return maps


def kernel(**inputs):
    inputs = {k_: np.asarray(v) for k_, v in inputs.items()}
    nc = build_program(S, stages=99)
    maps = make_in_maps(inputs, 8, S)
    res = run_bass_kernel_spmd(nc, maps, core_ids=list(range(8)))
    return np.stack([r["out"] for r in res.results], axis=0).astype(np.float32)
```
